# Optimizing a Trainium2 kernel written in Bass

```python
import math
import jax, jax.numpy as jnp
from jax import lax
import numpy as np

D_MODEL = 1024
BATCH = 8
SEQ = 2048
DEPTH = 2

CHUNK = 64
PLE_DIM = 256
CONV_WIDTH = 4
EPS = 1e-5
ROPE_BASE = 10000.0

MIX_HALF = D_MODEL // 2
MIX_OUT = 2 * MIX_HALF
LRU_WIDTH = MIX_HALF
LRU_BLOCKS = 8
LRU_BLOCK = LRU_WIDTH // LRU_BLOCKS
LRU_C = 8.0
RET_HEADS = 4
RET_DIM = MIX_HALF // RET_HEADS
SSD_WIDTH = MIX_HALF
SSD_HEADDIM = 64
SSD_HEADS = SSD_WIDTH // SSD_HEADDIM
SSD_GROUPS = 2
SSD_STATE = 128
SSD_XBC = SSD_WIDTH + 2 * SSD_GROUPS * SSD_STATE
HGRN_WIDTH = MIX_HALF
HGRN_HEADS = 4
HGRN_KDIM = HGRN_WIDTH // HGRN_HEADS
HGRN_VDIM = HGRN_WIDTH // HGRN_HEADS

EVEN_IN = 2 * LRU_WIDTH + 4 * MIX_HALF
ODD_IN = SSD_WIDTH + SSD_XBC + SSD_HEADS + 4 * HGRN_WIDTH

N_GROUPS = 4
EXPERTS_PER_GROUP = 4
N_EXPERTS = N_GROUPS * EXPERTS_PER_GROUP
EXPERT_FF = 256
TOP_K_IN_GROUP = 2

N_EVEN = (DEPTH + 1) // 2
N_ODD = DEPTH // 2
DEEPNORM_ALPHA = (2.0 * DEPTH) ** 0.25
DEEPNORM_BETA = (8.0 * DEPTH) ** -0.25

kernel_name = "hybrid_chunk_causal_trunk"


def layer_norm(x, g, b):
    xf = x.astype(jnp.float32)
    mu = jnp.mean(xf, -1, keepdims=True)
    var = jnp.mean(jnp.square(xf - mu), -1, keepdims=True)
    return ((xf - mu) * lax.rsqrt(var + EPS) * g + b).astype(x.dtype)


def rms_norm_groups(x, w, n_groups):
    shp = x.shape
    xf = x.astype(jnp.float32).reshape(shp[:-1] + (n_groups, shp[-1] // n_groups))
    xf = xf * lax.rsqrt(jnp.mean(jnp.square(xf), -1, keepdims=True) + EPS)
    return (xf.reshape(shp) * w).astype(x.dtype)


def head_group_norm(x, w, n_heads):
    shp = x.shape
    xf = x.astype(jnp.float32).reshape(shp[:-1] + (n_heads, shp[-1] // n_heads))
    mu = jnp.mean(xf, -1, keepdims=True)
    xf = (xf - mu) * lax.rsqrt(jnp.mean(jnp.square(xf - mu), -1, keepdims=True) + EPS)
    return xf.reshape(shp) * w


def causal_depthwise_conv(x, w, b):
    s = x.shape[1]
    xp = jnp.pad(x, ((0, 0), (CONV_WIDTH - 1, 0), (0, 0)))
    y = b
    for k in range(CONV_WIDTH):
        y = y + xp[:, k:k + s, :] * w[k]
    return y


def rotary(x, positions):
    d = x.shape[-1]
    inv = ROPE_BASE ** (-jnp.arange(0, d, 2, dtype=jnp.float32) / d)
    ang = positions.astype(jnp.float32)[..., None] * inv
    cos = jnp.cos(ang)[:, :, None, :]
    sin = jnp.sin(ang)[:, :, None, :]
    xf = x.astype(jnp.float32)
    x1, x2 = xf[..., :d // 2], xf[..., d // 2:]
    return jnp.concatenate([x1 * cos - x2 * sin, x2 * cos + x1 * sin], -1)


def to_chunks(x):
    bsz, s, h, d = x.shape
    return x.reshape(bsz, s // CHUNK, CHUNK, h, d).transpose(0, 3, 1, 2, 4)


def from_chunks(x):
    bsz, h, nc, c, d = x.shape
    return x.transpose(0, 2, 3, 1, 4).reshape(bsz, nc * c, h * d)


def rg_lru_mixer(xa, ya, conv_w, conv_b, w_r, b_r, w_i, b_i, lam):
    bsz, s, _ = xa.shape
    xc = causal_depthwise_conv(xa, conv_w, conv_b)
    xb = xc.reshape(bsz, s, LRU_BLOCKS, LRU_BLOCK)
    r = jax.nn.sigmoid(jnp.einsum('bshi,hij->bshj', xb, w_r).reshape(bsz, s, LRU_WIDTH) + b_r)
    gi = jax.nn.sigmoid(jnp.einsum('bshi,hij->bshj', xb, w_i).reshape(bsz, s, LRU_WIDTH) + b_i)
    log_a = -LRU_C * r.astype(jnp.float32) * jax.nn.softplus(-lam.astype(jnp.float32))
    a = jnp.exp(log_a)
    u = jnp.sqrt(-jnp.expm1(2.0 * log_a)) * (gi * xc).astype(jnp.float32)

    def combine(c1, c2):
        a1, b1 = c1
        a2, b2 = c2
        return a1 * a2, a2 * b1 + b2

    _, h = lax.associative_scan(combine, (a, u), axis=1)
    return (h.astype(xa.dtype) * jax.nn.gelu(ya)).astype(xa.dtype)


def retention_mixer(q, k, v, g, positions, gn_w):
    bsz, s, _ = q.shape
    q = rotary(q.reshape(bsz, s, RET_HEADS, RET_DIM), positions)
    k = rotary(k.reshape(bsz, s, RET_HEADS, RET_DIM), positions) * (RET_DIM ** -0.5)
    v = v.reshape(bsz, s, RET_HEADS, RET_DIM).astype(jnp.float32)
    qc, kc, vc = to_chunks(q), to_chunks(k), to_chunks(v)
    log_gamma = jnp.log1p(-(2.0 ** (-5.0 - jnp.arange(RET_HEADS, dtype=jnp.float32))))
    idx = jnp.arange(CHUNK, dtype=jnp.float32)
    decay_intra = jnp.exp(log_gamma[:, None, None] * jnp.abs(idx[:, None] - idx[None, :]))
    scores = jnp.einsum('bhnid,bhnjd->bhnij', qc, kc) * decay_intra[:, None]
    o_intra = jnp.einsum('bhnij,bhnje->bhnie', scores, vc)
    k_to_end = kc * jnp.exp(log_gamma[:, None] * (CHUNK - 1 - idx))[:, None, :, None]
    chunk_kv = jnp.einsum('bhnjd,bhnje->nbhde', k_to_end, vc)
    chunk_decay = jnp.exp(log_gamma * CHUNK)[:, None, None]

    def step(state, kv):
        return chunk_decay * state + kv, state

    _, prev = lax.scan(step, jnp.zeros_like(chunk_kv[0]), chunk_kv)
    q_from_start = qc * jnp.exp(log_gamma[:, None] * (idx + 1.0))[:, None, :, None]
    o_inter = jnp.einsum('bhnid,nbhde->bhnie', q_from_start, prev)
    o = head_group_norm(from_chunks(o_intra + o_inter), gn_w, RET_HEADS)
    return (jax.nn.silu(g.astype(jnp.float32)) * o).astype(g.dtype)


def ssd_mixer(z, xbc, dt, conv_w, conv_b, dt_bias, a_log, d_skip, norm_w):
    bsz, s, _ = z.shape
    xbc = jax.nn.silu(causal_depthwise_conv(xbc, conv_w, conv_b))
    xs = xbc[..., :SSD_WIDTH].reshape(bsz, s, SSD_HEADS, SSD_HEADDIM)
    bm = xbc[..., SSD_WIDTH:SSD_WIDTH + SSD_GROUPS * SSD_STATE].reshape(bsz, s, SSD_GROUPS, SSD_STATE)
    cm = xbc[..., SSD_WIDTH + SSD_GROUPS * SSD_STATE:].reshape(bsz, s, SSD_GROUPS, SSD_STATE)
    heads_per_group = SSD_HEADS // SSD_GROUPS
    bm = jnp.repeat(bm, heads_per_group, axis=2)
    cm = jnp.repeat(cm, heads_per_group, axis=2)
    dt = jax.nn.softplus(dt.astype(jnp.float32) + dt_bias)
    a = -jnp.exp(a_log.astype(jnp.float32))
    xc = to_chunks(xs.astype(jnp.float32))
    bc = to_chunks(bm.astype(jnp.float32))
    cc = to_chunks(cm.astype(jnp.float32))
    dtc = to_chunks(dt[..., None])[..., 0]
    cs = jnp.cumsum(dtc * a[None, :, None, None], axis=-1)
    causal = jnp.arange(CHUNK)[:, None] >= jnp.arange(CHUNK)[None, :]
    seg = jnp.exp(jnp.where(causal, cs[..., :, None] - cs[..., None, :], -jnp.inf))
    scores = jnp.einsum('bhnid,bhnjd->bhnij', cc, bc) * seg
    y_intra = jnp.einsum('bhnij,bhnjp->bhnip', scores, xc * dtc[..., None])
    decay_to_end = jnp.exp(cs[..., -1:] - cs)
    chunk_states = jnp.einsum('bhnjd,bhnjp->nbhpd', bc * (decay_to_end * dtc)[..., None], xc)
    chunk_decay = jnp.exp(cs[..., -1]).transpose(2, 0, 1)

    def step(state, inp):
        st, dec = inp
        return dec[..., None, None] * state + st, state

    _, prev = lax.scan(step, jnp.zeros_like(chunk_states[0]), (chunk_states, chunk_decay))
    y_inter = jnp.einsum('bhnid,nbhpd->bhnip', cc * jnp.exp(cs)[..., None], prev)
    y = from_chunks(y_intra + y_inter) + (xs.astype(jnp.float32) * d_skip[:, None]).reshape(bsz, s, SSD_WIDTH)
    y = y.astype(z.dtype)
    return rms_norm_groups(y * jax.nn.silu(z), norm_w, SSD_GROUPS)


def hgrn2_mixer(q, f_raw, i_in, g, lower_bound, norm_w):
    bsz, s, _ = q.shape
    qf = jax.nn.silu(q.astype(jnp.float32))
    f = lower_bound + (1.0 - lower_bound) * jax.nn.sigmoid(f_raw.astype(jnp.float32))
    kf = 1.0 - f
    shp = (bsz, s, HGRN_HEADS, HGRN_KDIM)
    qc = to_chunks(qf.reshape(shp))
    kc = to_chunks(kf.reshape(shp))
    bc = jnp.cumsum(to_chunks(jnp.log(f).reshape(shp)), axis=3)
    vc = to_chunks(i_in.astype(jnp.float32).reshape(bsz, s, HGRN_HEADS, HGRN_VDIM))
    xs = tuple(t.transpose(2, 0, 1, 3, 4) for t in (qc, kc, vc, bc))
    causal = jnp.arange(CHUNK)[:, None] >= jnp.arange(CHUNK)[None, :]

    def step(state, inp):
        qn, kn, vn, bn = inp
        rel = bn[:, :, :, None, :] - bn[:, :, None, :, :]
        w = jnp.exp(jnp.where(causal[..., None], rel, -jnp.inf))
        scores = jnp.einsum('bhik,bhjk,bhijk->bhij', qn, kn, w)
        o = jnp.einsum('bhij,bhjv->bhiv', scores, vn) + jnp.einsum('bhik,bhkv->bhiv', qn * jnp.exp(bn), state)
        b_end = bn[:, :, -1:, :]
        new_state = jnp.exp(b_end[:, :, 0, :])[..., None] * state + jnp.einsum('bhjk,bhjv->bhkv', kn * jnp.exp(b_end - bn), vn)
        return new_state, o

    state0 = jnp.zeros((bsz, HGRN_HEADS, HGRN_KDIM, HGRN_VDIM), jnp.float32)
    _, o = lax.scan(step, state0, xs)
    o = from_chunks(o.transpose(1, 2, 0, 3, 4)).astype(q.dtype)
    return (rms_norm_groups(o, norm_w, HGRN_HEADS) * jax.nn.silu(g)).astype(q.dtype)


def hierarchical_moe(x, w_group, b_group, w_router, b_router, w1, w3, w2):
    bsz, s, d = x.shape
    t = x.reshape(bsz * s, d)
    group_logits = (t @ w_group + b_group).astype(jnp.float32)
    group_probs = jax.nn.softmax(group_logits, -1)
    g_sel = jnp.argmax(group_logits, -1)
    p_group = jnp.max(group_probs, -1, keepdims=True)
    g_onehot = jax.nn.one_hot(g_sel, N_GROUPS, dtype=jnp.float32)
    expert_logits = (jnp.einsum('td,gde->tge', t, w_router) + b_router).astype(jnp.float32)
    in_group = jnp.einsum('tge,tg->te', expert_logits, g_onehot)
    top_val, top_idx = lax.top_k(in_group, TOP_K_IN_GROUP)
    top_w = jax.nn.softmax(top_val, -1) * p_group
    expert_id = g_sel[:, None] * EXPERTS_PER_GROUP + top_idx
    gate = jnp.sum(jax.nn.one_hot(expert_id, N_EXPERTS, dtype=jnp.float32) * top_w[..., None], axis=1)
    h = jax.nn.silu(jnp.einsum('td,edf->tef', t, w1)) * jnp.einsum('td,edf->tef', t, w3)
    y = jnp.einsum('tef,efd->td', h * gate[..., None].astype(h.dtype), w2)
    return y.reshape(bsz, s, d).astype(x.dtype)


def setup_inputs(seed: int = 0) -> dict:
    key = jax.random.key(seed)
    ks = iter(jax.random.split(key, 48))

    def nrm(shape, scale):
        return jax.random.normal(next(ks), shape, jnp.float32) * scale

    def ones_noise(shape):
        return 1.0 + nrm(shape, 0.01)

    x = nrm((BATCH, SEQ, D_MODEL), 1.0)
    p = nrm((DEPTH, BATCH, SEQ, PLE_DIM), 1.0)
    offsets = jax.random.randint(next(ks), (BATCH, 1), 0, 64, dtype=jnp.int32) * CHUNK
    positions = (offsets + jnp.arange(SEQ, dtype=jnp.int32)[None, :]).astype(jnp.int32)
    hgrn_lb_logits = nrm((DEPTH, HGRN_WIDTH), 0.5)

    ev_w_in = nrm((N_EVEN, D_MODEL, EVEN_IN), D_MODEL ** -0.5)
    ev_w_out = nrm((N_EVEN, MIX_OUT, D_MODEL), MIX_OUT ** -0.5 * DEEPNORM_BETA)
    lru_conv_w = nrm((N_EVEN, CONV_WIDTH, LRU_WIDTH), CONV_WIDTH ** -0.5)
    lru_conv_b = nrm((N_EVEN, LRU_WIDTH), 0.01)
    lru_w_r = nrm((N_EVEN, LRU_BLOCKS, LRU_BLOCK, LRU_BLOCK), LRU_BLOCK ** -0.5)
    lru_b_r = nrm((N_EVEN, LRU_WIDTH), 0.01)
    lru_w_i = nrm((N_EVEN, LRU_BLOCKS, LRU_BLOCK, LRU_BLOCK), LRU_BLOCK ** -0.5)
    lru_b_i = nrm((N_EVEN, LRU_WIDTH), 0.01)
    u = jax.random.uniform(next(ks), (N_EVEN, LRU_WIDTH), jnp.float32, 0.9, 0.999)
    a0 = u ** (1.0 / LRU_C)
    lru_lambda = jnp.log(a0) - jnp.log1p(-a0)
    ret_gn_w = ones_noise((N_EVEN, MIX_HALF))

    od_w_in = nrm((N_ODD, D_MODEL, ODD_IN), D_MODEL ** -0.5)
    od_w_out = nrm((N_ODD, MIX_OUT, D_MODEL), MIX_OUT ** -0.5 * DEEPNORM_BETA)
    ssd_conv_w = nrm((N_ODD, CONV_WIDTH, SSD_XBC), CONV_WIDTH ** -0.5)
    ssd_conv_b = nrm((N_ODD, SSD_XBC), 0.01)
    dt0 = jnp.exp(jax.random.uniform(next(ks), (N_ODD, SSD_HEADS), jnp.float32, math.log(1e-3), math.log(1e-1)))
    ssd_dt_bias = dt0 + jnp.log(-jnp.expm1(-dt0))
    ssd_a_log = jnp.log(jax.random.uniform(next(ks), (N_ODD, SSD_HEADS), jnp.float32, 1.0, 16.0))
    ssd_d = ones_noise((N_ODD, SSD_HEADS))
    ssd_norm_w = ones_noise((N_ODD, SSD_WIDTH))
    hgrn_norm_w = ones_noise((N_ODD, HGRN_WIDTH))

    ln_mix_g = ones_noise((DEPTH, D_MODEL))
    ln_mix_b = nrm((DEPTH, D_MODEL), 0.01)
    ln_ffn_g = ones_noise((DEPTH, D_MODEL))
    ln_ffn_b = nrm((DEPTH, D_MODEL), 0.01)

    moe_w_group = nrm((DEPTH, D_MODEL, N_GROUPS), D_MODEL ** -0.5)
    moe_b_group = nrm((DEPTH, N_GROUPS), 0.01)
    moe_w_router = nrm((DEPTH, N_GROUPS, D_MODEL, EXPERTS_PER_GROUP), D_MODEL ** -0.5)
    moe_b_router = nrm((DEPTH, N_GROUPS, EXPERTS_PER_GROUP), 0.01)
    moe_w1 = nrm((DEPTH, N_EXPERTS, D_MODEL, EXPERT_FF), D_MODEL ** -0.5)
    moe_w3 = nrm((DEPTH, N_EXPERTS, D_MODEL, EXPERT_FF), D_MODEL ** -0.5)
    moe_w2 = nrm((DEPTH, N_EXPERTS, EXPERT_FF, D_MODEL), EXPERT_FF ** -0.5 * DEEPNORM_BETA)

    ple_w_proj = nrm((DEPTH, PLE_DIM, D_MODEL), PLE_DIM ** -0.5)
    ple_w_gate = nrm((DEPTH, D_MODEL, D_MODEL), D_MODEL ** -0.5)

    return {"x": x, "p": p, "positions": positions, "hgrn_lb_logits": hgrn_lb_logits,
            "ev_w_in": ev_w_in, "ev_w_out": ev_w_out, "lru_conv_w": lru_conv_w, "lru_conv_b": lru_conv_b,
            "lru_w_r": lru_w_r, "lru_b_r": lru_b_r, "lru_w_i": lru_w_i, "lru_b_i": lru_b_i,
            "lru_lambda": lru_lambda, "ret_gn_w": ret_gn_w,
            "od_w_in": od_w_in, "od_w_out": od_w_out, "ssd_conv_w": ssd_conv_w, "ssd_conv_b": ssd_conv_b,
            "ssd_dt_bias": ssd_dt_bias, "ssd_a_log": ssd_a_log, "ssd_d": ssd_d, "ssd_norm_w": ssd_norm_w,
            "hgrn_norm_w": hgrn_norm_w,
            "ln_mix_g": ln_mix_g, "ln_mix_b": ln_mix_b, "ln_ffn_g": ln_ffn_g, "ln_ffn_b": ln_ffn_b,
            "moe_w_group": moe_w_group, "moe_b_group": moe_b_group, "moe_w_router": moe_w_router,
            "moe_b_router": moe_b_router, "moe_w1": moe_w1, "moe_w3": moe_w3, "moe_w2": moe_w2,
            "ple_w_proj": ple_w_proj, "ple_w_gate": ple_w_gate}


def reference(x, p, positions, hgrn_lb_logits,
              ev_w_in, ev_w_out, lru_conv_w, lru_conv_b, lru_w_r, lru_b_r, lru_w_i, lru_b_i,
              lru_lambda, ret_gn_w,
              od_w_in, od_w_out, ssd_conv_w, ssd_conv_b, ssd_dt_bias, ssd_a_log, ssd_d, ssd_norm_w,
              hgrn_norm_w,
              ln_mix_g, ln_mix_b, ln_ffn_g, ln_ffn_b,
              moe_w_group, moe_b_group, moe_w_router, moe_b_router, moe_w1, moe_w3, moe_w2,
              ple_w_proj, ple_w_gate):
    lb_soft = jax.nn.softmax(hgrn_lb_logits.astype(jnp.float32), axis=0)
    lb_all = jnp.cumsum(lb_soft, axis=0) - lb_soft[0]
    h = x
    for i in range(DEPTH):
        j = i // 2
        if i % 2 == 0:
            u = h @ ev_w_in[j]
            xa, ya, q, k, v, g = jnp.split(u, [LRU_WIDTH, 2 * LRU_WIDTH, 2 * LRU_WIDTH + MIX_HALF,
                                               2 * LRU_WIDTH + 2 * MIX_HALF, 2 * LRU_WIDTH + 3 * MIX_HALF], axis=-1)
            out_a = rg_lru_mixer(xa, ya, lru_conv_w[j], lru_conv_b[j], lru_w_r[j], lru_b_r[j],
                                 lru_w_i[j], lru_b_i[j], lru_lambda[j])
            out_b = retention_mixer(q, k, v, g, positions, ret_gn_w[j])
            mix = jnp.concatenate([out_a, out_b], axis=-1) @ ev_w_out[j]
        else:
            u = h @ od_w_in[j]
            c0 = SSD_WIDTH
            c1 = c0 + SSD_XBC
            c2 = c1 + SSD_HEADS
            z, xbc, dt, q, f_raw, i_in, g = jnp.split(u, [c0, c1, c2, c2 + HGRN_WIDTH, c2 + 2 * HGRN_WIDTH,
                                                          c2 + 3 * HGRN_WIDTH], axis=-1)
            out_c = ssd_mixer(z, xbc, dt, ssd_conv_w[j], ssd_conv_b[j], ssd_dt_bias[j], ssd_a_log[j],
                              ssd_d[j], ssd_norm_w[j])
            out_d = hgrn2_mixer(q, f_raw, i_in, g, lb_all[i], hgrn_norm_w[j])
            mix = jnp.concatenate([out_c, out_d], axis=-1) @ od_w_out[j]
        h = layer_norm(DEEPNORM_ALPHA * h + mix, ln_mix_g[i], ln_mix_b[i])
        moe = hierarchical_moe(h, moe_w_group[i], moe_b_group[i], moe_w_router[i], moe_b_router[i],
                               moe_w1[i], moe_w3[i], moe_w2[i])
        h = layer_norm(DEEPNORM_ALPHA * h + moe, ln_ffn_g[i], ln_ffn_b[i])
        h = (h + (p[i] @ ple_w_proj[i]) * jax.nn.sigmoid(h @ ple_w_gate[i])).astype(x.dtype)
    return h
```

```python
import math
from contextlib import contextmanager, ExitStack
import numpy as np
import concourse.bass as bass
import concourse.mybir as mybir
from concourse.bass_utils import run_bass_kernel_spmd

F32 = mybir.dt.float32
BF16 = mybir.dt.bfloat16
I32 = mybir.dt.int32
AF = mybir.ActivationFunctionType
ALU = mybir.AluOpType
AX = mybir.AxisListType

D = 1024
SEQ = 2048
NCORES = 8
DEPTH = 2
ALPHA = (2.0 * DEPTH) ** 0.25
EPS = 1e-5
NT = SEQ // 512
NTT = SEQ // 128


class T:
    def __init__(self, h, nslots=1, name=""):
        self.h = h
        self.name = name
        self.n = nslots
        self.last_w = [None] * nslots
        self.readers = [dict() for _ in range(nslots)]
        self.excl = False

    def v(self, ap=None, slots=None):
        if ap is None and self.h is not None and slots is None:
            ap = self.h[:]
        if slots is None:
            slots = range(self.n)
        elif isinstance(slots, int):
            slots = (slots,)
        return V(self, ap, tuple(slots))


class V:
    __slots__ = ("t", "ap", "slots")

    def __init__(self, t, ap, slots):
        self.t = t
        self.ap = ap
        self.slots = slots


class Eng:
    def __init__(self, name, handle, sem):
        self.name = name
        self.e = handle
        self.sem = sem
        self.count = 0
        self.known = {}
        self.snaps = [None]


class K:
    NDMA = 24

    def __init__(self):
        nc = bass.Bass("TRN2", target_bir_lowering=False)
        self.nc = nc
        self.eng = {}
        for nm, h in (("pe", nc.tensor), ("act", nc.scalar), ("dve", nc.vector),
                      ("pool", nc.gpsimd), ("sp", nc.sync)):
            self.eng[nm] = Eng(nm, h, nc.alloc_semaphore("s_" + nm))
        self.dsem = [nc.alloc_semaphore("s_dma%d" % i) for i in range(self.NDMA)]
        self.dcnt = [0] * self.NDMA
        self.dsnap = [[None] for _ in range(self.NDMA)]
        self.dnext = 0
        self.dnext_sw = 0
        self.nwaits = 0
        self.ninst = 0
        self._ps = []
        self._psi = 0
        self.uid = 0
        self._stack = None
        self.marks = []

    def sb(self, shape, dtype=F32, nslots=1, name=None):
        self.uid += 1
        name = "%s_%d" % (name or "t", self.uid)
        if self._stack is not None:
            h = self._stack.enter_context(self.nc.sbuf_tensor(name, list(shape), dtype))
            return T(h, nslots, name)
        return T(self.nc.alloc_sbuf_tensor(name, list(shape), dtype), nslots, name)

    @contextmanager
    def phase(self):
        old = self._stack
        st = ExitStack()
        self._stack = st
        try:
            yield
        finally:
            self.barrier()
            st.close()
            self._stack = old

    def psum_pool(self, n=8):
        for i in range(n):
            h = self.nc.alloc_psum_tensor("psb%d" % i, [128, 512], F32)
            t = T(h, 1, "psb%d" % i)
            t.excl = True
            self._ps.append(t)

    def ps(self):
        t = self._ps[self._psi % len(self._ps)]
        self._psi += 1
        return t

    def dram(self, name, shape, dtype=F32, kind="Internal"):
        return self.nc.dram_tensor(name, list(shape), dtype, kind=kind).ap()

    def _sem_of(self, pname):
        if pname[0] == "d" and pname[1:].isdigit():
            return self.dsem[int(pname[1:])], 16
        return self.eng[pname].sem, 1

    def _snap_of(self, pname, seq):
        if pname[0] == "d" and pname[1:].isdigit():
            return self.dsnap[int(pname[1:])][seq]
        return self.eng[pname].snaps[seq]

    def _collect(self, reads, writes):
        deps = {}
        raw = {}
        for a in reads:
            for sl in a.slots:
                p = a.t.last_w[sl]
                if p is not None:
                    if deps.get(p[0], 0) < p[1]:
                        deps[p[0]] = p[1]
                    if raw.get(p[0], 0) < p[1]:
                        raw[p[0]] = p[1]
                if a.t.excl:
                    for nm, s in a.t.readers[sl].items():
                        if deps.get(nm, 0) < s:
                            deps[nm] = s
        for a in writes:
            for sl in a.slots:
                p = a.t.last_w[sl]
                if p is not None and deps.get(p[0], 0) < p[1]:
                    deps[p[0]] = p[1]
                for nm, s in a.t.readers[sl].items():
                    if deps.get(nm, 0) < s:
                        deps[nm] = s
        return deps, raw

    def _wait(self, eng, nm, s):
        if eng.known.get(nm, 0) >= s:
            return
        sem, mult = self._sem_of(nm)
        eng.e.wait_ge(sem, s * mult)
        self.nwaits += 1
        snap = self._snap_of(nm, s)
        if snap:
            for k2, v2 in snap.items():
                if eng.known.get(k2, 0) < v2:
                    eng.known[k2] = v2
        if eng.known.get(nm, 0) < s:
            eng.known[nm] = s

    def _wait_deps(self, eng, deps, raw, same_all=False):
        for nm, s in deps.items():
            if nm == eng.name:
                if eng.name in ("pe", "sp"):
                    continue
                if not same_all:
                    s = raw.get(nm, 0)
                    if s == 0:
                        continue
            self._wait(eng, nm, s)

    def _mark(self, pname, seq, reads, writes):
        for a in reads:
            for sl in a.slots:
                r = a.t.readers[sl]
                if r.get(pname, 0) < seq:
                    r[pname] = seq
        for a in writes:
            for sl in a.slots:
                a.t.last_w[sl] = (pname, seq)
                a.t.readers[sl] = {}

    def op(self, ename, fn, reads=(), writes=()):
        eng = self.eng[ename]
        deps, raw = self._collect(reads, writes)
        self._wait_deps(eng, deps, raw)
        ins = fn(eng.e)
        eng.count += 1
        ins.then_inc(eng.sem, 1)
        eng.snaps.append(dict(eng.known))
        self._mark(ename, eng.count, reads, writes)
        self.ninst += 1
        return ins

    def dma(self, qname, out, in_, reads=(), writes=(), **kw):
        eng = self.eng[qname]
        deps, raw = self._collect(reads, writes)
        self._wait_deps(eng, deps, raw, same_all=True)
        half = self.NDMA // 2
        if qname == "pool":
            di = half + self.dnext_sw
            self.dnext_sw = (self.dnext_sw + 1) % half
        else:
            di = self.dnext
            self.dnext = (self.dnext + 1) % half
        pname = "d%d" % di
        if self.dcnt[di] > 0:
            self._wait(eng, pname, self.dcnt[di])
        ins = eng.e.dma_start(out=out, in_=in_, **kw)
        ins.then_inc(self.dsem[di], 16)
        self.dcnt[di] += 1
        self.dsnap[di].append(dict(eng.known))
        self._mark(pname, self.dcnt[di], reads, writes)
        self.ninst += 1
        return ins

    def mark(self, name):
        self.marks.append((name, {n: e.count for n, e in self.eng.items()}))

    def barrier(self):
        names = list(self.eng.keys())
        for nm in names:
            eng = self.eng[nm]
            for nm2 in names:
                if self.eng[nm2].count > 0 and (nm2 != nm or nm in ("act", "dve", "pool")):
                    self._wait(eng, nm2, self.eng[nm2].count)
            for i in range(self.NDMA):
                if self.dcnt[i] > 0:
                    self._wait(eng, "d%d" % i, self.dcnt[i])

    def finish(self):
        sp = self.eng["sp"]
        for nm, e in self.eng.items():
            if nm != "sp" and e.count > 0:
                self._wait(sp, nm, e.count)
        for i in range(self.NDMA):
            if self.dcnt[i] > 0:
                self._wait(sp, "d%d" % i, self.dcnt[i])

    def mm(self, out, lhsT, rhs, start=True, stop=True):
        return self.op("pe", lambda e: e.matmul(out.ap, lhsT=lhsT.ap, rhs=rhs.ap, start=start, stop=stop),
                       reads=[lhsT, rhs], writes=[out])

    def tr(self, out, in_, ident):
        return self.op("pe", lambda e: e.transpose(out.ap, in_.ap, ident.ap),
                       reads=[in_, ident], writes=[out])

    def act(self, out, in_, func, bias=None, scale=1.0):
        reads = [in_]
        kw = {}
        if isinstance(bias, V):
            reads.append(bias)
            kw["bias"] = bias.ap
        elif bias is not None:
            kw["bias"] = bias
        if isinstance(scale, V):
            reads.append(scale)
            kw["scale"] = scale.ap
        else:
            kw["scale"] = scale
        return self.op("act", lambda e: e.activation(out=out.ap, in_=in_.ap, func=func, **kw),
                       reads=reads, writes=[out])

    def tt(self, out, in0, in1, op, eng="dve"):
        return self.op(eng, lambda e: e.tensor_tensor(out=out.ap, in0=in0.ap, in1=in1.ap, op=op),
                       reads=[in0, in1], writes=[out])

    def ts(self, out, in0, s1, op0, s2=None, op1=None, eng="dve"):
        reads = [in0]
        a1 = s1
        a2 = s2
        if isinstance(s1, V):
            reads.append(s1)
            a1 = s1.ap
        if isinstance(s2, V):
            reads.append(s2)
            a2 = s2.ap
        if op1 is None:
            return self.op(eng, lambda e: e.tensor_scalar(out=out.ap, in0=in0.ap, scalar1=a1, scalar2=None, op0=op0),
                           reads=reads, writes=[out])
        return self.op(eng, lambda e: e.tensor_scalar(out=out.ap, in0=in0.ap, scalar1=a1, scalar2=a2, op0=op0, op1=op1),
                       reads=reads, writes=[out])

    def stt(self, out, in0, scalar, in1, op0, op1):
        reads = [in0, in1]
        a = scalar
        if isinstance(scalar, V):
            reads.append(scalar)
            a = scalar.ap
        return self.op("dve", lambda e: e.scalar_tensor_tensor(out=out.ap, in0=in0.ap, scalar=a, in1=in1.ap, op0=op0, op1=op1),
                       reads=reads, writes=[out])

    def copy(self, out, in_, eng="dve"):
        if eng == "act":
            return self.act(out, in_, AF.Copy)
        return self.op(eng, lambda e: e.tensor_copy(out=out.ap, in_=in_.ap), reads=[in_], writes=[out])

    def memset(self, out, val, eng="dve"):
        return self.op(eng, lambda e: e.memset(out.ap, val), reads=[], writes=[out])


RET_GAMMA_LOG = [math.log1p(-(2.0 ** (-5.0 - h))) for h in range(4)]


class PP:
    def __init__(self):
        self.cols = {}
        self.arrs = []
        self.n = 0

    def add(self, name, v):
        v = np.asarray(v, dtype=np.float32).reshape(-1)
        assert v.size % 128 == 0, (name, v.size)
        n = v.size // 128
        self.arrs.append(np.ascontiguousarray(v.reshape(n, 128).T))
        self.cols[name] = (self.n, n)
        self.n += n

    def build(self):
        return np.ascontiguousarray(np.concatenate(self.arrs, axis=1))


def make_pp(inp):
    pp = PP()
    for i in range(DEPTH):
        pp.add("ln_mix_g%d" % i, inp["ln_mix_g"][i])
        pp.add("ln_mix_b%d" % i, inp["ln_mix_b"][i])
        pp.add("ln_ffn_g%d" % i, inp["ln_ffn_g"][i])
        pp.add("ln_ffn_b%d" % i, inp["ln_ffn_b"][i])
    for kk in range(4):
        pp.add("lru_cw%d" % kk, inp["lru_conv_w"][0][kk])
    pp.add("lru_cb", inp["lru_conv_b"][0])
    pp.add("lru_br", inp["lru_b_r"][0])
    pp.add("lru_bi", inp["lru_b_i"][0])
    pp.add("lru_lam", inp["lru_lambda"][0])
    pp.add("ret_gnw", inp["ret_gn_w"][0])
    for kk in range(4):
        pp.add("ssd_cw%d" % kk, inp["ssd_conv_w"][0][kk])
    pp.add("ssd_cb", inp["ssd_conv_b"][0])
    pp.add("ssd_nw", inp["ssd_norm_w"][0])
    pp.add("hgrn_nw", inp["hgrn_norm_w"][0])
    pp.add("ssd_dcol", np.repeat(np.asarray(inp["ssd_d"][0], dtype=np.float32), 64))
    pp.add("hgrn_lb0", inp["hgrn_lb_logits"][0])
    pp.add("hgrn_lb1", inp["hgrn_lb_logits"][1])
    d = np.arange(128)
    inv = (10000.0 ** (-(np.arange(0, 128, 2, dtype=np.float32)) / 128.0)).astype(np.float32)
    pp.add("inv_freq", inv[d % 64])
    pp.add("sin_sign", np.where(d < 64, -1.0, 1.0))
    for h in range(4):
        pp.add("kdec%d" % h, np.exp(RET_GAMMA_LOG[h] * (127.0 - d)) * (128.0 ** -0.5))
    return pp


def make_consts():
    c = {}
    c["ident"] = np.eye(128, dtype=np.float32)
    j = np.arange(128)[:, None]
    i = np.arange(128)[None, :]
    cj, ci = j // 64, i // 64
    m = np.zeros((4, 128, 128), np.float32)
    for h in range(4):
        lg = RET_GAMMA_LOG[h]
        same = np.exp(lg * np.abs(i - j))
        prev = np.exp(lg * (i - j))
        m[h] = np.where(cj == ci, same, np.where(cj < ci, prev, 0.0)) * (128.0 ** -0.5)
    c["retmask"] = m
    qd = np.zeros((4, 512), np.float32)
    for h in range(4):
        qd[h] = np.tile(np.exp(RET_GAMMA_LOG[h] * (np.arange(128) + 1.0)), 4)
    c["qdec"] = qd
    cm = ((i >= j) & (cj == ci)).astype(np.float32)
    c["cmask"] = cm
    cm128 = (i >= j).astype(np.float32)
    c["cmask128"] = cm128
    sel = np.zeros((16, 16 * 128), np.float32)
    for e in range(16):
        sel[e, e * 128:(e + 1) * 128] = 1.0
    c["sel"] = sel
    c["tri"] = ((j <= i) & (cj == ci)).astype(np.float32)
    c["tri128"] = (j <= i).astype(np.float32)
    pm = np.zeros((128, 128), np.float32)
    for m_ in range(128):
        pm[(m_ + 64) % 128, m_] = 1.0
    c["perm"] = pm
    return c


WEIGHT_INPUTS = [
    ("ev_w_in", [1024, 3072]), ("ev_w_out", [1024, 1024]),
    ("od_w_in", [1024, 3592]), ("od_w_out", [1024, 1024]),
    ("lru_w_r", [8, 64, 64]), ("lru_w_i", [8, 64, 64]),
    ("moe_w_group", [2, 1024, 4]), ("moe_w_router", [2, 4, 1024, 4]),
    ("moe_w1", [2, 16, 1024, 256]), ("moe_w3", [2, 16, 1024, 256]), ("moe_w2", [2, 16, 256, 1024]),
    ("ple_w_proj", [2, 256, 1024]), ("ple_w_gate", [2, 1024, 1024]),
    ("moe_bias", [2, 20]),
    ("ssd_hp", [3, 8]),
]


class Prog:
    def __init__(self, layers=(0, 1), dbg=None, first_from_x=True, last_to_out=True):
        self.layers = layers
        self.dbg = dbg or {}
        k = self.k = K()
        nc = k.nc
        self.d = {}
        dd = self.d
        dd["x"] = k.dram("x", [SEQ, D], F32, "ExternalInput")
        dd["p"] = k.dram("p", [DEPTH, SEQ, 256], F32, "ExternalInput")
        dd["pos"] = k.dram("pos", [1, SEQ], I32, "ExternalInput")
        for nm, shp in WEIGHT_INPUTS:
            dd[nm] = k.dram(nm, shp, F32, "ExternalInput")
        self.ppinfo = None

    def setup(self, pp_cols, npp, consts_shapes):
        k = self.k
        dd = self.d
        self.pc = pp_cols
        dd["pp"] = k.dram("pp", [128, npp], F32, "ExternalInput")
        for nm, shp in consts_shapes.items():
            dd[nm] = k.dram(nm, list(shp), F32, "ExternalInput")
        dd["out"] = k.dram("out", [SEQ, D], F32, "ExternalOutput")
        for nm, (shp, dt_) in self.dbg.items():
            dd[nm] = k.dram(nm, list(shp), dt_, "ExternalOutput")
        k.psum_pool(8)
        self.R = None
        self.cosT = None
        self.sinT = None
        self.rot_done = False
        dd["r_spill"] = k.dram("r_spill", [128, 8, SEQ], F32)
        self.Rd_t = T(None, 4, "r_spill")
        self.HB = k.sb([128, 8, SEQ], BF16, 32, "HB")
        self.MIX = k.sb([128, 8, SEQ], BF16, 32, "MIX")
        self.pp = k.sb([128, npp + 64], F32, 1, "pp")
        self.ppx = npp
        self.ident = k.sb([128, 128], F32, 1, "ident")
        self.identb = k.sb([128, 128], BF16, 1, "identb")
        self.ones = {}
        self.epsv = k.sb([128, 1], F32, 1, "epsv")
        k.memset(self.epsv.v(), EPS)
        k.dma("sp", self.pp.h[:, 0:npp], dd["pp"], writes=[self.pp.v()])
        k.dma("sp", self.ident.h[:], dd["ident"], writes=[self.ident.v()])
        k.copy(self.identb.v(), self.ident.v())
        for nm, val in (("o1024", 1.0 / 1024), ("o128", 1.0 / 128), ("o256", 1.0 / 256), ("one", 1.0)):
            t = k.sb([128, 128], BF16, 1, nm)
            k.memset(t.v(), val)
            self.ones[nm] = t

    def alloc_R(self):
        self.R = self.k.sb([128, 8, SEQ], F32, 32, "R")

    def spill_R(self, j, q="sp"):
        sl = [self.rs(c, j) for c in range(8)]
        self.k.dma(q, self.d["r_spill"][:, :, j * 512:(j + 1) * 512], self.R.h[:, :, j * 512:(j + 1) * 512],
                   reads=[self.R.v(None, sl)], writes=[V(self.Rd_t, None, (j,))])

    def reload_R(self):
        for j in range(NT):
            sl = [self.rs(c, j) for c in range(8)]
            self.k.dma("sp", self.R.h[:, :, j * 512:(j + 1) * 512], self.d["r_spill"][:, :, j * 512:(j + 1) * 512],
                       reads=[V(self.Rd_t, None, (j,))], writes=[self.R.v(None, sl)])

    def pcol(self, name, j=0, n=1):
        c0, nn = self.pc[name]
        return self.pp.v(self.pp.h[:, c0 + j:c0 + j + n])

    def xcol(self, j, n=1):
        return self.pp.v(self.pp.h[:, self.ppx + j:self.ppx + j + n])

    def rs(self, c, j):
        return c * 4 + j

    def Rv(self, c, j):
        return self.R.v(self.R.h[:, c, j * 512:(j + 1) * 512], self.rs(c, j))

    def HBv(self, c, j):
        return self.HB.v(self.HB.h[:, c, j * 512:(j + 1) * 512], self.rs(c, j))

    def MIXv(self, c, j):
        return self.MIX.v(self.MIX.h[:, c, j * 512:(j + 1) * 512], self.rs(c, j))

    def dump(self, name, src_v, dram_ap=None):
        if name in self.dbg:
            self.k.dma("sp", dram_ap if dram_ap is not None else self.d[name], src_v.ap, reads=[src_v])

    def load_x(self, scale, write_R=True, write_HB=True):
        for _ in self.load_x_gen(scale, write_R, write_HB):
            pass

    def load_x_gen(self, scale, write_R=True, write_HB=True):
        k = self.k
        xt = [k.sb([128, D], F32, 1, "xtok") for _ in range(4)]
        for tt in range(NTT):
            t = xt[tt % 4]
            k.dma("sp", t.h[:], self.d["x"][tt * 128:(tt + 1) * 128, :], writes=[t.v()])
            for g in range(2):
                ps = k.ps()
                for q in range(4):
                    c = g * 4 + q
                    k.tr(ps.v(ps.h[:, q * 128:(q + 1) * 128]), t.v(t.h[:, c * 128:(c + 1) * 128]), self.ident.v())
                j = tt // 4
                sl = [self.rs(g * 4 + q, j) for q in range(4)]
                t0 = tt * 128
                pv = ps.v(ps.h[:].rearrange("p (a b) -> p a b", a=4))
                first_act = (tt * 2 + g) % 2 == 0
                if write_HB:
                    hbv = self.HB.v(self.HB.h[:, g * 4:g * 4 + 4, t0:t0 + 128], sl)
                    if first_act:
                        k.act(hbv, pv, AF.Copy)
                    else:
                        k.copy(hbv, pv)
                    first_act = not first_act
                if write_R:
                    rv = self.R.v(self.R.h[:, g * 4:g * 4 + 4, t0:t0 + 128], sl)
                    if first_act:
                        k.act(rv, pv, AF.Copy, scale=scale)
                    else:
                        k.ts(rv, pv, scale, ALU.mult)
            yield

    def load_w(self, dst, dram2d, eng="pool"):
        k = self.k
        k.dma(eng, dst.h[:], dram2d.rearrange("(kc p) n -> p kc n", p=128), writes=[dst.v()])

    def proj(self, wt, col0, j, ncols=128, kcs=8, src=None):
        k = self.k
        ps = k.ps()
        for kc in range(kcs):
            rhs = self.HBv(kc, j) if src is None else src(kc, j)
            k.mm(ps.v(ps.h[0:ncols, :]), wt.v(wt.h[:, kc, col0:col0 + ncols]), rhs, start=(kc == 0), stop=(kc == kcs - 1))
        return ps

    def mixer_lru(self):
        for _ in self.mixer_lru_gen():
            pass

    def mixer_lru_gen(self):
        k = self.k
        dd = self.d
        W = dd["ev_w_in"]
        w_xa = k.sb([128, 8, 128], BF16, 1, "w_xa")
        w_ya = k.sb([128, 8, 128], BF16, 1, "w_ya")
        wr = k.sb([128, 4, 128], BF16, 1, "wbd_r")
        wi = k.sb([128, 4, 128], BF16, 1, "wbd_i")
        k.memset(wr.v(), 0.0)
        k.memset(wi.v(), 0.0)
        for dst, src in ((wr, dd["lru_w_r"]), (wi, dd["lru_w_i"])):
            for cc in range(4):
                for hb in range(2):
                    k.dma("pool", dst.h[hb * 64:(hb + 1) * 64, cc, hb * 64:(hb + 1) * 64], src[cc * 2 + hb],
                          writes=[dst.v()])
        lam = self.pcol("lru_lam", 0, 4)
        tA = k.sb([128, 4], F32, 1, "tA")
        tB = k.sb([128, 4], F32, 1, "tB")
        nsp = k.sb([128, 4], F32, 1, "nsp")
        k.ts(tB.v(), lam, -1.0, ALU.mult, 0.0, ALU.max)
        k.stt(tA.v(), tB.v(), 2.0, lam, ALU.mult, ALU.add)
        k.act(tA.v(), tA.v(), AF.Exp, scale=-1.0)
        k.act(tA.v(), tA.v(), AF.Ln, bias=1.0)
        k.tt(tB.v(), tB.v(), tA.v(), ALU.add)
        k.ts(nsp.v(), tB.v(), -8.0, ALU.mult)

        xa = k.sb([128, SEQ], F32, 4, "xa")
        gy = k.sb([128, SEQ], BF16, 4, "gy")
        xc = k.sb([128, SEQ], F32, 1, "xc")
        xcb = k.sb([128, SEQ], BF16, 1, "xcb")
        a_t = k.sb([128, SEQ], F32, 4, "a_t")
        u_t = k.sb([128, SEQ], F32, 4, "u_t")
        hh = k.sb([128, SEQ], F32, 1, "hh")

        def sl(t, j):
            return t.v(t.h[:, j * 512:(j + 1) * 512], j)

        def ldw(dst, c0):
            k.dma("pool", dst.h[:], W[:, c0:c0 + 128].rearrange("(kc p) n -> p kc n", p=128), writes=[dst.v()])

        for cc in range(4):
            ldw(w_xa, cc * 128)
            ldw(w_ya, 512 + cc * 128)
            for j in range(NT):
                ps = self.proj(w_xa, 0, j)
                k.act(sl(xa, j), ps.v(), AF.Copy)
                yield
            for j in range(NT):
                ps = self.proj(w_ya, 0, j)
                k.act(sl(gy, j), ps.v(), AF.Gelu_apprx_tanh)
                yield
            cw = [self.pcol("lru_cw%d" % kk, cc) for kk in range(4)]
            cb = self.pcol("lru_cb", cc)
            k.ts(xc.v(), xa.v(), cw[3], ALU.mult, cb, ALU.add)
            yield
            for kk, sh in ((2, 1), (1, 2), (0, 3)):
                k.stt(xc.v(xc.h[:, sh:SEQ]), xa.v(xa.h[:, 0:SEQ - sh]), cw[kk], xc.v(xc.h[:, sh:SEQ]), ALU.mult, ALU.add)
                yield
            k.copy(xcb.v(), xc.v(), eng="pool")
            for j in range(NT):
                ps = k.ps()
                k.mm(ps.v(), wr.v(wr.h[:, cc, :]), xcb.v(xcb.h[:, j * 512:(j + 1) * 512]))
                k.act(sl(a_t, j), ps.v(), AF.Sigmoid, bias=self.pcol("lru_br", cc))
                ps = k.ps()
                k.mm(ps.v(), wi.v(wi.h[:, cc, :]), xcb.v(xcb.h[:, j * 512:(j + 1) * 512]))
                k.act(sl(u_t, j), ps.v(), AF.Sigmoid, bias=self.pcol("lru_bi", cc))
                yield
            k.act(a_t.v(), a_t.v(), AF.Exp, scale=nsp.v(nsp.h[:, cc:cc + 1]))
            yield
            k.tt(hh.v(), a_t.v(), a_t.v(), ALU.mult)
            k.act(hh.v(), hh.v(), AF.Sqrt, bias=1.0, scale=-1.0)
            yield
            k.tt(u_t.v(), u_t.v(), xc.v(), ALU.mult, eng="pool")
            k.tt(u_t.v(), u_t.v(), hh.v(), ALU.mult)
            yield
            k.op("dve", lambda e: e.tensor_tensor_scan(out=hh.h[:], data0=a_t.h[:], data1=u_t.h[:], initial=0.0,
                                                       op0=ALU.mult, op1=ALU.add),
                 reads=[a_t.v(), u_t.v()], writes=[hh.v()])
            yield
            k.tt(self.MIX.v(self.MIX.h[:, cc, :], [self.rs(cc, j) for j in range(4)]), hh.v(), gy.v(), ALU.mult, eng="pool")
            yield


def host_arrays(inp):
    pp = make_pp(inp)
    consts = make_consts()
    shared = {}
    f = lambda a: np.ascontiguousarray(np.asarray(a, dtype=np.float32))
    shared["ev_w_in"] = f(inp["ev_w_in"][0])
    shared["ev_w_out"] = f(inp["ev_w_out"][0])
    shared["od_w_in"] = f(inp["od_w_in"][0])
    shared["od_w_out"] = f(inp["od_w_out"][0])
    shared["lru_w_r"] = f(inp["lru_w_r"][0])
    shared["lru_w_i"] = f(inp["lru_w_i"][0])
    for nm in ("moe_w_group", "moe_w_router", "moe_w1", "moe_w3", "moe_w2", "ple_w_proj", "ple_w_gate"):
        shared[nm] = f(inp[nm])
    shared["moe_bias"] = f(np.concatenate([np.asarray(inp["moe_b_group"]).reshape(2, 4),
                                           np.asarray(inp["moe_b_router"]).reshape(2, 16)], axis=1))
    shared["ssd_hp"] = f(np.stack([np.asarray(inp["ssd_dt_bias"][0]), np.asarray(inp["ssd_a_log"][0]),
                                   np.asarray(inp["ssd_d"][0])], axis=0))
    shared["pp"] = pp.build()
    for nm, a in consts.items():
        shared[nm] = a
    return shared, pp, consts


def core_arrays(inp, b):
    return {
        "x": np.ascontiguousarray(np.asarray(inp["x"][b], dtype=np.float32)),
        "p": np.ascontiguousarray(np.asarray(inp["p"][:, b], dtype=np.float32)),
        "pos": np.ascontiguousarray(np.asarray(inp["positions"][b], dtype=np.int32).reshape(1, SEQ)),
    }


TWO_PI = 2.0 * math.pi
CW1 = 6.28125
CW2 = TWO_PI - CW1
PI_SAFE = 3.1415925


def _prog_method(f):
    setattr(Prog, f.__name__, f)
    return f


@_prog_method
def rotary_tables(self):
    for _ in self.rotary_gen():
        pass


@_prog_method
def rotary_gen(self):
    k = self.k
    HS = SEQ // 4
    if self.cosT is None:
        self.cosT = k.sb([128, SEQ], F32, 4, "cosT")
        self.sinT = k.sb([128, SEQ], F32, 4, "sinT")
    posi = k.sb([128, HS], I32, 1, "posi")
    ang = k.sb([128, HS], F32, 1, "ang")
    a2 = k.sb([128, HS], F32, 1, "a2")
    kf = k.sb([128, HS], F32, 1, "kf")
    for half in range(4):
        hs = slice(half * HS, (half + 1) * HS)
        k.dma("sp", posi.h[:], self.d["pos"][:, hs].partition_broadcast(128), writes=[posi.v()])
        k.copy(ang.v(), posi.v())
        k.ts(ang.v(), ang.v(), self.pcol("inv_freq"), ALU.mult)
        yield
        for shift, dst_t in ((math.pi / 2.0, self.cosT), (0.0, self.sinT)):
            dst = dst_t.v(dst_t.h[:, hs], half)
            if shift != 0.0:
                k.ts(a2.v(), ang.v(), shift, ALU.add)
                src = a2
            else:
                src = ang
            k.ts(kf.v(), src.v(), 1.0 / TWO_PI, ALU.mult)
            k.copy(posi.v(), kf.v())
            k.copy(kf.v(), posi.v())
            yield
            k.stt(dst, kf.v(), -CW1, src.v(), ALU.mult, ALU.add)
            k.stt(dst, kf.v(), -CW2, dst, ALU.mult, ALU.add)
            yield
            k.ts(kf.v(), dst, math.pi, ALU.is_gt, -TWO_PI, ALU.mult)
            k.tt(dst, dst, kf.v(), ALU.add)
            k.ts(kf.v(), dst, -math.pi, ALU.is_lt, TWO_PI, ALU.mult)
            k.tt(dst, dst, kf.v(), ALU.add)
            yield
            k.ts(dst, dst, PI_SAFE, ALU.min, -PI_SAFE, ALU.max)
            k.act(dst, dst, AF.Sin)
            if dst_t is self.sinT:
                k.ts(dst, dst, self.pcol("sin_sign"), ALU.mult)
            yield


@_prog_method
def mixer_ret(self):
    for _ in self.mixer_ret_gen():
        pass


@_prog_method
def mixer_ret_gen(self):
    k = self.k
    dd = self.d
    W = dd["ev_w_in"]
    if not self.rot_done:
        yield from self.rotary_gen()
    maskT = k.sb([128, 4, 128], F32, 1, "retmask")
    k.dma("sp", maskT.h[:], dd["retmask"].rearrange("h j i -> j h i"), writes=[maskT.v()])
    qdec = k.sb([128, 4, 128], F32, 1, "qdec")
    k.dma("sp", qdec.h[:], dd["qdec"][:, 0:128].rearrange("(o h) n -> o h n", o=1).partition_broadcast(128),
          writes=[qdec.v()])
    vtok = k.sb([128, NTT, 128], BF16, NTT, "vtok")
    wv = k.sb([128, 8, 128], BF16, 1, "wv")
    wq = k.sb([128, 8, 128], BF16, 1, "wq")
    permf = k.sb([128, 128], F32, 1, "permf")
    permb = k.sb([128, 128], BF16, 1, "permb")
    k.dma("sp", permf.h[:], dd["perm"], writes=[permf.v()])
    k.copy(permb.v(), permf.v())
    xb16 = [k.sb([128, 512], BF16, 1, "xb16")] * 2
    wk = k.sb([128, 8, 128], BF16, 1, "wk")
    wg = k.sb([128, 8, 128], BF16, 1, "wg")
    qr = k.sb([128, SEQ], BF16, 4, "qr")
    qfs = k.sb([128, SEQ], BF16, 4, "qfs")
    kr = k.sb([128, SEQ], BF16, 4, "kr")
    kte = k.sb([128, NTT, 128], BF16, NTT, "kte")
    sg = (k.sb([128, 512], BF16, 1, "sgj"), wg)
    o_ts = [k.sb([128, 512], F32, 1, "o_t")] * 2
    ob = k.sb([128, 512], BF16, 1, "ob")
    osq = k.sb([128, 512], BF16, 1, "osq")
    t1 = [k.sb([128, 512], F32, 1, "t1") for _ in range(2)]
    t2 = [k.sb([128, 512], F32, 1, "t2") for _ in range(2)]
    t3 = [k.sb([128, 512], F32, 1, "t3")] * 2
    scm4 = [k.sb([128, 4, 128], BF16, 1, "scm4") for _ in range(2)]
    kvall = k.sb([128, NTT, 128], F32, NTT, "ret_kvall")
    Sball = k.sb([128, NTT - 1, 128], BF16, 1, "ret_Sball")

    def sl(t, j):
        return t.v(t.h[:, j * 512:(j + 1) * 512], j)

    def ldw(dst, c0):
        k.dma("pool", dst.h[:], W[:, c0:c0 + 128].rearrange("(kc p) n -> p kc n", p=128), writes=[dst.v()])

    def ldw_swapped(dst, c0):
        for a, b in ((0, 64), (64, 0)):
            k.dma("pool", dst.h[:, :, a:a + 64], W[:, c0 + b:c0 + b + 64].rearrange("(kc p) n -> p kc n", p=128),
                  writes=[dst.v()])

    it = 0
    for hd in range(4):
        ldw(wq, 1024 + hd * 128)
        ldw(wk, 1536 + hd * 128)
        ldw(wg, 2560 + hd * 128)
        ldw(wv, 2048 + hd * 128)
        for tt in range(NTT):
            ps = k.ps()
            for kc in range(8):
                k.mm(ps.v(ps.h[:, 0:128]), self.HB.v(self.HB.h[:, kc, tt * 128:(tt + 1) * 128], self.rs(kc, tt // 4)),
                     wv.v(wv.h[:, kc, :]), start=(kc == 0), stop=(kc == 7))
            k.copy(vtok.v(vtok.h[:, tt, :], tt), ps.v(ps.h[:, 0:128]), eng="act")
            if tt % 2 == 1:
                yield
        for j in range(NT):
            cs = self.cosT.v(self.cosT.h[:, j * 512:(j + 1) * 512], j)
            sn = self.sinT.v(self.sinT.h[:, j * 512:(j + 1) * 512], j)
            for (w0, dst) in ((wq, qr), (wk, kr)):
                a, b = t1[it % 2], t2[it % 2]
                it += 1
                ps = self.proj(w0, 0, j)
                xb = xb16[it % 2]
                k.copy(xb.v(), ps.v(), eng="act")
                k.tt(a.v(), ps.v(), cs, ALU.mult)
                ps2 = k.ps()
                k.mm(ps2.v(), permb.v(), xb.v())
                k.tt(b.v(), ps2.v(), sn, ALU.mult)
                k.tt(sl(dst, j), a.v(), b.v(), ALU.add, eng="pool")
                if dst is qr:
                    k.tt(a.v(), a.v(), b.v(), ALU.add, eng="pool")
                    k.tt(qfs.v(qfs.h[:, j * 512:(j + 1) * 512].rearrange("p (a b) -> p a b", a=4), j),
                         a.v(a.h[:].rearrange("p (a b) -> p a b", a=4)),
                         qdec.v(qdec.h[:, hd:hd + 1, :].to_broadcast([128, 4, 128])), ALU.mult, eng="pool")
                yield
        for tt in range(NTT):
            ps = k.ps()
            pb = ps.v(ps.h[:].bitcast(BF16)[:, 0:128])
            k.tr(pb, kr.v(kr.h[:, tt * 128:(tt + 1) * 128], tt // 4), self.identb.v())
            k.ts(kte.v(kte.h[:, tt, :], tt), pb, self.pcol("kdec%d" % hd), ALU.mult)
            if tt % 4 == 3:
                yield
        g128 = math.exp(RET_GAMMA_LOG[hd] * 128.0)
        for q4 in range(NTT // 4):
            pk = k.ps()
            tts = [tt for tt in range(q4 * 4, q4 * 4 + 4) if tt < NTT - 1]
            for tt in tts:
                r_ = tt % 4
                k.mm(pk.v(pk.h[:, r_ * 128:(r_ + 1) * 128]), kte.v(kte.h[:, tt, :], tt), vtok.v(vtok.h[:, tt, :], tt))
            n_ = len(tts)
            k.copy(kvall.v(kvall.h[:, q4 * 4:q4 * 4 + n_, :], tts),
                   pk.v(pk.h[:, 0:n_ * 128].rearrange("p (a b) -> p a b", a=n_)), eng="act")
            yield
        for tt in range(1, NTT - 1):
            k.stt(kvall.v(kvall.h[:, tt, :], tt), kvall.v(kvall.h[:, tt - 1, :], tt - 1), g128,
                  kvall.v(kvall.h[:, tt, :], tt), ALU.mult, ALU.add)
        k.copy(Sball.v(), kvall.v(kvall.h[:, 0:NTT - 1, :], range(NTT - 1)), eng="pool")
        yield
        for j4 in range(NT):
            ps = k.ps()
            for r_ in range(4):
                tt = j4 * 4 + r_
                tok = slice(tt * 128, (tt + 1) * 128)
                k.mm(ps.v(ps.h[:, r_ * 128:(r_ + 1) * 128]), kr.v(kr.h[:, tok], j4), qr.v(qr.h[:, tok], j4))
            sc = scm4[j4 % 2]
            k.tt(sc.v(), ps.v(ps.h[:].rearrange("p (a b) -> p a b", a=4)),
                 maskT.v(maskT.h[:, hd:hd + 1, :].to_broadcast([128, 4, 128])), ALU.mult)
            po = k.ps()
            for r_ in range(4):
                tt = j4 * 4 + r_
                tok = slice(tt * 128, (tt + 1) * 128)
                reg = po.v(po.h[:, r_ * 128:(r_ + 1) * 128])
                k.mm(reg, vtok.v(vtok.h[:, tt, :], tt), sc.v(sc.h[:, r_, :]), start=True, stop=(tt == 0))
                if tt > 0:
                    k.mm(reg, Sball.v(Sball.h[:, tt - 1, :]), qfs.v(qfs.h[:, tok], j4), start=False, stop=True)
            o_t = o_ts[j4 % 2]
            k.copy(o_t.v(), po.v(), eng="act")
            self._ret_gn(hd, j4, o_t, sg, ob, osq, t1, t2, t3)
            yield


@_prog_method
def _ret_gn(self, hd, j, o_t, sg, ob, osq, t1, t2, t3):
    k = self.k
    oj = o_t.v()
    k.copy(ob.v(), oj, eng="act")
    k.act(osq.v(), oj, AF.Square)
    pm = k.ps()
    k.mm(pm.v(), self.ones["o128"].v(), ob.v())
    pq = k.ps()
    k.mm(pq.v(), self.ones["o128"].v(), osq.v())
    a, b, c = t1[j % 2], t2[j % 2], t3[j % 2]
    k.act(a.v(), pm.v(), AF.Square)
    k.tt(b.v(), pq.v(), a.v(), ALU.subtract)
    k.act(b.v(), b.v(), AF.Ln, bias=self.epsv.v())
    k.act(b.v(), b.v(), AF.Exp, scale=-0.5)
    k.tt(c.v(), oj, pm.v(), ALU.subtract)
    k.tt(c.v(), c.v(), b.v(), ALU.mult, eng="pool")
    sgj, wg = sg
    ps = self.proj(wg, 0, j)
    k.act(sgj.v(), ps.v(), AF.Silu)
    k.stt(self.MIXv(4 + hd, j), c.v(), self.pcol("ret_gnw", hd), sgj.v(), ALU.mult, ALU.mult)


@_prog_method
def derived_params(self):
    k = self.k
    for i in range(DEPTH):
        for w, nm in enumerate(("ln_mix_g%d" % i, "ln_mix_b%d" % i)):
            k.ts(self.xcol(i * 16 + w * 8, 8), self.pcol(nm, 0, 8), ALPHA, ALU.mult)


@_prog_method
def ln_stats(self, j, yb, ysq, tmp):
    k = self.k
    yb, ysq, tmp = yb[j % len(yb)], ysq[j % len(ysq)], tmp[j % len(tmp)]
    for c in range(8):
        k.copy(yb.v(yb.h[:, c, :], c), self.Rv(c, j), eng=("dve" if c % 2 == 0 else "pool"))
        k.act(ysq.v(ysq.h[:, c, :], c), self.Rv(c, j), AF.Square)
    pm = k.ps()
    for c in range(8):
        k.mm(pm.v(), self.ones["o1024"].v(), yb.v(yb.h[:, c, :], c), start=(c == 0), stop=(c == 7))
    pq = k.ps()
    for c in range(8):
        k.mm(pq.v(), self.ones["o1024"].v(), ysq.v(ysq.h[:, c, :], c), start=(c == 0), stop=(c == 7))
    m2, rstd, nmr, t = tmp
    k.act(m2.v(), pm.v(), AF.Square)
    k.tt(rstd.v(), pq.v(), m2.v(), ALU.subtract)
    k.act(rstd.v(), rstd.v(), AF.Ln, bias=self.epsv.v())
    k.act(rstd.v(), rstd.v(), AF.Exp, scale=-0.5)
    k.stt(nmr.v(), pm.v(), -1.0, rstd.v(), ALU.mult, ALU.mult)


@_prog_method
def ln_apply(self, j, g, b, ga, ba, tmp):
    k = self.k
    m2, rstd, nmr, t = tmp[j % len(tmp)]
    for c in range(8):
        tc_ = t[c % 2]
        k.tt(tc_.v(), self.Rv(c, j), rstd.v(), ALU.mult)
        k.tt(tc_.v(), tc_.v(), nmr.v(), ALU.add, eng="pool")
        k.ts(self.Rv(c, j), tc_.v(), ga(c), ALU.mult, ba(c), ALU.add)
        k.act(self.HBv(c, j), tc_.v(), AF.Identity, bias=b(c), scale=g(c))


@_prog_method
def ln_tile(self, j, g, b, ga, ba, yb, ysq, tmp):
    self.ln_stats(j, yb, ysq, tmp)
    self.ln_apply(j, g, b, ga, ba, tmp)


@_prog_method
def ln_bufs(self, nbuf=2, ntmp=1):
    k = self.k
    yb = [k.sb([128, 8, 512], BF16, 8, "yb") for _ in range(nbuf)]
    ysq = [k.sb([128, 8, 512], BF16, 8, "ysq") for _ in range(nbuf)]
    tmp = [(k.sb([128, 512], F32, 1, "m2"), k.sb([128, 512], F32, 1, "rstd"), k.sb([128, 512], F32, 1, "nmr"),
            [k.sb([128, 512], F32, 1, "lnt") for _ in range(2)]) for _ in range(ntmp)]
    return yb, ysq, tmp


@_prog_method
def load_wout(self, i):
    k = self.k
    Wd = self.d["ev_w_out" if i % 2 == 0 else "od_w_out"]
    self.wo = k.sb([128, 8, 1024], BF16, 1, "w_out")
    for h2 in range(2):
        k.dma("pool", self.wo.h[:, :, h2 * 512:(h2 + 1) * 512],
              Wd[:, h2 * 512:(h2 + 1) * 512].rearrange("(kc p) n -> p kc n", p=128), writes=[self.wo.v()])


@_prog_method
def outproj_ln(self, i):
    k = self.k
    wo = self.wo
    yb, ysq, tmp = self.ln_bufs(2, 2)
    g = lambda c: self.pcol("ln_mix_g%d" % i, c)
    b = lambda c: self.pcol("ln_mix_b%d" % i, c)
    ga = lambda c: self.xcol(i * 16 + c)
    ba = lambda c: self.xcol(i * 16 + 8 + c)
    for j in range(NT + 2):
        if j < NT:
            for c in range(8):
                ps = self.proj(wo, c * 128, j, src=self.MIXv)
                k.tt(self.Rv(c, j), self.Rv(c, j), ps.v(), ALU.add)
        if 0 < j <= NT:
            self.ln_stats(j - 1, yb, ysq, tmp)
        if j > 1:
            self.ln_apply(j - 2, g, b, ga, ba, tmp)
            self.router_logits(j - 2)


@_prog_method
def ln_ffn(self, i):
    k = self.k
    yb, ysq, tmp = self.ln_bufs()
    g = lambda c: self.pcol("ln_ffn_g%d" % i, c)
    b = lambda c: self.pcol("ln_ffn_b%d" % i, c)
    for j in range(NT):
        self.ln_tile(j, g, b, g, b, yb, ysq, tmp)


@_prog_method
def router_setup(self, i):
    k = self.k
    dd = self.d
    wr = k.sb([128, 8, 20], F32, 1, "wr32")
    k.dma("sp", wr.h[:, :, 0:4], dd["moe_w_group"][i].rearrange("(kc p) e -> p kc e", p=128), writes=[wr.v()])
    for g in range(4):
        k.dma("sp", wr.h[:, :, 4 + 4 * g:8 + 4 * g], dd["moe_w_router"][i, g].rearrange("(kc p) e -> p kc e", p=128),
              writes=[wr.v()])
    bias = k.sb([128, 20], F32, 1, "rbias")
    k.dma("sp", bias.h[:], dd["moe_bias"][i:i + 1, :].partition_broadcast(128), writes=[bias.v()])
    L = k.sb([128, NTT, 20], F32, NT, "L")
    self.router_st = (wr, bias, L)


@_prog_method
def router_logits(self, j):
    k = self.k
    wr, bias, L = self.router_st
    for tt in range(4 * j, 4 * j + 4):
        ps = k.ps()
        for kc in range(8):
            k.mm(ps.v(ps.h[:, 0:20]), self.R.v(self.R.h[:, kc, tt * 128:(tt + 1) * 128], self.rs(kc, tt // 4)),
                 wr.v(wr.h[:, kc, :]), start=(kc == 0), stop=(kc == 7))
        k.stt(L.v(L.h[:, tt, :], j), ps.v(ps.h[:, 0:20]), 1.0 / ALPHA, bias.v(), ALU.mult, ALU.add)


@_prog_method
def moe_route(self, i, gT):
    k = self.k
    dd = self.d
    wr, bias, L = self.router_st
    N = NTT
    GL = L.h[:, :, 0:4]
    EL = L.h[:, :, 4:20].rearrange("p t (g e) -> p t g e", g=4)
    mk = lambda shape, nm: k.sb(shape, F32, 1, nm)
    gmax = mk([128, N], "gmax")
    oh = mk([128, N, 4], "oh")
    ge = mk([128, N, 4], "ge")
    gs = mk([128, N], "gs")
    tmp4 = mk([128, N, 4, 4], "tmp4")
    ing = mk([128, N, 4], "ing")
    m1 = mk([128, N], "m1")
    k1 = mk([128, N, 4], "k1")
    ing2 = mk([128, N, 4], "ing2")
    m2 = mk([128, N], "m2")
    k2 = mk([128, N, 4], "k2")
    ed = mk([128, N], "ed")
    w1 = mk([128, N], "w1")
    w2 = mk([128, N], "w2")
    gate = mk([128, N, 4, 4], "gate")
    Lv = L.v()

    def b3(t):
        return t.v(t.h[:, :].unsqueeze(2).to_broadcast([128, N, 4]))

    def red(out, in_ap, in_t, op):
        k.op("dve", lambda e: e.tensor_reduce(out=out.h[:], in_=in_ap, axis=AX.X, op=op), reads=[in_t.v()], writes=[out.v()])

    red(gmax, GL, L, ALU.max)
    k.tt(oh.v(), L.v(GL), b3(gmax), ALU.is_equal)
    k.tt(ge.v(), L.v(GL), b3(gmax), ALU.subtract)
    k.act(ge.v(), ge.v(), AF.Exp)
    red(gs, ge.h[:], ge, ALU.add)
    k.op("dve", lambda e: e.reciprocal(out=gs.h[:], in_=gs.h[:]), reads=[gs.v()], writes=[gs.v()])
    k.tt(tmp4.v(), L.v(EL), oh.v(oh.h[:, :, :].unsqueeze(3).to_broadcast([128, N, 4, 4])), ALU.mult)
    red(ing, tmp4.h[:].rearrange("p t g e -> p t e g"), tmp4, ALU.add)
    red(m1, ing.h[:], ing, ALU.max)
    k.tt(k1.v(), ing.v(), b3(m1), ALU.is_equal)
    k.stt(ing2.v(), k1.v(), -1.0e30, ing.v(), ALU.mult, ALU.add)
    red(m2, ing2.h[:], ing2, ALU.max)
    k.tt(k2.v(), ing2.v(), b3(m2), ALU.is_equal)
    k.tt(ed.v(), m2.v(), m1.v(), ALU.subtract)
    k.act(ed.v(), ed.v(), AF.Exp)
    k.ts(w1.v(), ed.v(), 1.0, ALU.add)
    k.op("dve", lambda e: e.reciprocal(out=w1.h[:], in_=w1.h[:]), reads=[w1.v()], writes=[w1.v()])
    k.tt(w2.v(), ed.v(), w1.v(), ALU.mult)
    k.tt(w1.v(), w1.v(), gs.v(), ALU.mult)
    k.tt(w2.v(), w2.v(), gs.v(), ALU.mult)
    k.tt(k1.v(), k1.v(), b3(w1), ALU.mult)
    k.tt(k2.v(), k2.v(), b3(w2), ALU.mult)
    k.tt(k1.v(), k1.v(), k2.v(), ALU.add)
    k.tt(gate.v(), oh.v(oh.h[:, :, :].unsqueeze(3).to_broadcast([128, N, 4, 4])),
         k1.v(k1.h[:, :, :].unsqueeze(2).to_broadcast([128, N, 4, 4])), ALU.mult)
    for g4 in range(NTT // 4):
        ps = k.ps()
        for q in range(4):
            tt = g4 * 4 + q
            k.tr(ps.v(ps.h[0:16, q * 128:(q + 1) * 128]),
                 gate.v(gate.h[:, tt, :, :].rearrange("p g e -> p (g e)")), self.ident.v())
        k.copy(gT.v(gT.h[0:16, g4 * 512:(g4 + 1) * 512]), ps.v(ps.h[0:16, :]), eng="act")
    self.dump("dbg_gate", gate.v(), None)


@_prog_method
def moe(self, i):
    k = self.k
    dd = self.d
    gT = k.sb([16, SEQ], BF16, 1, "gT")
    selb = k.sb([16, 16 * 128], BF16, 1, "selb")
    w1s = [k.sb([128, 8, 256], BF16, 1, "w1e") for _ in range(2)]
    w3s = [k.sb([128, 8, 256], BF16, 1, "w3e") for _ in range(2)]
    w2s = [k.sb([128, 2, 1024], BF16, 1, "w2e") for _ in range(2)]
    NB = 4
    gB = [k.sb([128, 512], F32, 1, "gB") for _ in range(2)]
    s1 = [k.sb([128, 512], F32, 1, "s1") for _ in range(2)]
    tm = [k.sb([128, 512], F32, 1, "tm") for _ in range(2)]
    hm = [k.sb([128, 2, 512], BF16, 2, "hm") for _ in range(NB)]
    ysb = [k.sb([128, 512], F32, 1, "ysb") for _ in range(3)]

    def load_expert(e):
        w1e, w3e, w2e = w1s[e % 2], w3s[e % 2], w2s[e % 2]
        k.dma("pool", w1e.h[:], dd["moe_w1"][i, e].rearrange("(kc p) f -> p kc f", p=128), writes=[w1e.v()])
        k.dma("pool", w3e.h[:], dd["moe_w3"][i, e].rearrange("(kc p) f -> p kc f", p=128), writes=[w3e.v()])
        k.dma("pool", w2e.h[:], dd["moe_w2"][i, e].rearrange("(fc p) n -> p fc n", p=128), writes=[w2e.v()])

    def stage_a(e, j, it, part):
        w1e, w3e = w1s[e % 2], w3s[e % 2]
        gb = gB[it % 2]
        hme = hm[it % NB]
        if part == 0:
            pg = k.ps()
            k.mm(pg.v(), selb.v(selb.h[0:16, e * 128:(e + 1) * 128]), gT.v(gT.h[0:16, j * 512:(j + 1) * 512]))
            k.copy(gb.v(), pg.v(), eng="act")
        f = part
        p1 = self.proj(w1e, f * 128, j)
        p3 = self.proj(w3e, f * 128, j)
        k.act(s1[f].v(), p1.v(), AF.Silu)
        k.tt(tm[f].v(), s1[f].v(), p3.v(), ALU.mult)
        k.tt(hme.v(hme.h[:, f, :], f), tm[f].v(), gb.v(), ALU.mult, eng="pool")

    def stage_b(e, j, it, part):
        w2e = w2s[e % 2]
        hme = hm[it % NB]
        for c in range(4 * part, 4 * part + 4):
            py = k.ps()
            k.mm(py.v(), w2e.v(w2e.h[:, 0, c * 128:(c + 1) * 128]), hme.v(hme.h[:, 0, :], 0), start=True, stop=False)
            k.mm(py.v(), w2e.v(w2e.h[:, 1, c * 128:(c + 1) * 128]), hme.v(hme.h[:, 1, :], 1), start=False, stop=True)
            if c not in (1, 4, 7):
                k.tt(self.Rv(c, j), self.Rv(c, j), py.v(), ALU.add)
            else:
                yb_ = ysb[(c // 3) % len(ysb)]
                k.copy(yb_.v(), py.v(), eng="act")
                k.tt(self.Rv(c, j), self.Rv(c, j), yb_.v(), ALU.add, eng="pool")

    steps = [(e, j) for e in range(16) for j in range(NT)]
    load_expert(0)
    load_expert(1)
    k.dma("pool", selb.h[:], dd["sel"], writes=[selb.v()])
    with k.phase():
        self.moe_route(i, gT)
    for it, (e, j) in enumerate(steps):
        stage_a(e, j, it, 0)
        if it > 0:
            stage_b(steps[it - 1][0], steps[it - 1][1], it - 1, 0)
        stage_a(e, j, it, 1)
        if it > 0:
            stage_b(steps[it - 1][0], steps[it - 1][1], it - 1, 1)
        if j == 0 and 1 <= e < 15:
            load_expert(e + 1)
    stage_b(steps[-1][0], steps[-1][1], len(steps) - 1, 0)
    stage_b(steps[-1][0], steps[-1][1], len(steps) - 1, 1)


@_prog_method
def ple_prep(self, i):
    k = self.k
    dd = self.d
    PT = k.sb([128, 2, SEQ], BF16, 4, "PT")
    pt = [k.sb([128, 256], F32, 1, "ptok") for _ in range(2)]
    for g4 in range(NTT // 4):
        pss = [k.ps(), k.ps()]
        for q in range(4):
            tt = g4 * 4 + q
            t = pt[tt % 2]
            k.dma("sp", t.h[:], dd["p"][i, tt * 128:(tt + 1) * 128, :], writes=[t.v()])
            for kc in range(2):
                k.tr(pss[kc].v(pss[kc].h[:, q * 128:(q + 1) * 128]), t.v(t.h[:, kc * 128:(kc + 1) * 128]), self.ident.v())
        for kc in range(2):
            k.copy(PT.v(PT.h[:, kc, g4 * 512:(g4 + 1) * 512], g4), pss[kc].v(), eng="act")
    wg = k.sb([128, 8, 1024], BF16, 1, "w_pg")
    wp = k.sb([128, 2, 1024], BF16, 1, "w_pp")
    for h2 in range(2):
        k.dma("pool", wg.h[:, :, h2 * 512:(h2 + 1) * 512],
              dd["ple_w_gate"][i][:, h2 * 512:(h2 + 1) * 512].rearrange("(kc p) n -> p kc n", p=128), writes=[wg.v()])
    k.dma("pool", wp.h[:], dd["ple_w_proj"][i].rearrange("(kc p) n -> p kc n", p=128), writes=[wp.v()])
    sg = [k.sb([128, 512], F32, 1, "psg") for _ in range(2)]
    tq = [k.sb([128, 512], F32, 1, "ptq") for _ in range(2)]
    return PT, wg, wp, sg, tq


@_prog_method
def ple_tile(self, j, st, aout, write_hb):
    k = self.k
    PT, wg, wp, sg, tq = st
    for c in range(8):
        pgt = self.proj(wg, c * 128, j)
        pp_ = k.ps()
        for kc in range(2):
            k.mm(pp_.v(), wp.v(wp.h[:, kc, c * 128:(c + 1) * 128]), PT.v(PT.h[:, kc, j * 512:(j + 1) * 512], j),
                 start=(kc == 0), stop=(kc == 1))
        s_, t_ = sg[c % 2], tq[c % 2]
        k.act(s_.v(), pgt.v(), AF.Sigmoid)
        k.stt(t_.v(), s_.v(), aout, pp_.v(), ALU.mult, ALU.mult)
        k.stt(self.Rv(c, j), self.Rv(c, j), aout, t_.v(), ALU.mult, ALU.add)
    if write_hb:
        for c in range(8):
            k.act(self.HBv(c, j), self.Rv(c, j), AF.Copy, scale=1.0 / aout)
        self.spill_R(j)


@_prog_method
def ln_ffn_ple(self, i, aout, write_hb):
    k = self.k
    st = self.ple_prep(i)
    yb, ysq, tmp = self.ln_bufs(1, 2)
    g = lambda c: self.pcol("ln_ffn_g%d" % i, c)
    b = lambda c: self.pcol("ln_ffn_b%d" % i, c)
    for j in range(NT + 2):
        if j < NT:
            self.ln_stats(j, yb, ysq, tmp)
        if 0 < j <= NT:
            self.ln_apply(j - 1, g, b, g, b, tmp)
        if j > 1:
            self.ple_tile(j - 2, st, aout, write_hb)


@_prog_method
def store_out(self):
    k = self.k
    ot = [k.sb([128, D], F32, 1, "otok") for _ in range(2)]
    for tt in range(NTT):
        t = ot[tt % 2]
        for g in range(2):
            ps = k.ps()
            for q in range(4):
                c = g * 4 + q
                k.tr(ps.v(ps.h[:, q * 128:(q + 1) * 128]),
                     self.R.v(self.R.h[:, c, tt * 128:(tt + 1) * 128], self.rs(c, tt // 4)), self.ident.v())
            if g == 0:
                k.copy(t.v(t.h[:, 0:512]), ps.v(), eng="act")
            else:
                k.copy(t.v(t.h[:, 512:1024]), ps.v())
        k.dma("sp", self.d["out"][tt * 128:(tt + 1) * 128, :], t.h[:], reads=[t.v()])


@_prog_method
def layer(self, i, last):
    self.layer_mixers(i)
    self.layer_rest(i, last)


@_prog_method
def layer_mixers(self, i):
    k = self.k
    with k.phase():
        self.mixers(i)
    k.mark("L%d mixers" % i)


@_prog_method
def layer_rest(self, i, last):
    k = self.k
    with k.phase():
        self.alloc_R()
        self.router_setup(i)
        with k.phase():
            self.load_wout(i)
            if i == 0:
                with k.phase():
                    self.load_x(ALPHA, write_R=True, write_HB=False)
            else:
                self.reload_R()
            with k.phase():
                self.outproj_ln(i)
        k.mark("L%d outproj_ln" % i)
        with k.phase():
            self.moe(i)
        k.mark("L%d moe" % i)
        with k.phase():
            self.ln_ffn_ple(i, 1.0 if last else ALPHA, not last)
        k.mark("L%d ln_ffn_ple" % i)
        if last:
            with k.phase():
                self.store_out()
            k.mark("store")


def run_interleaved(gens):
    gens = list(gens)
    while gens:
        for g in list(gens):
            try:
                next(g)
            except StopIteration:
                gens.remove(g)


@_prog_method
def mixers(self, i):
    if i % 2 == 0:
        run_interleaved([self.mixer_ret_gen(), self.mixer_lru_gen()])
    else:
        k = self.k
        with k.phase():
            st = self.ssd_pre()
            run_interleaved([self.ssd_main_gen(st)])
        with k.phase():
            run_interleaved([self.mixer_hgrn_gen()])


@_prog_method
def softplus_inplace(self, x, tmp, tmp2):
    k = self.k
    k.ts(tmp.v(), x.v(), -1.0, ALU.mult, 0.0, ALU.max)
    k.stt(tmp2.v(), tmp.v(), 2.0, x.v(), ALU.mult, ALU.add)
    k.act(tmp2.v(), tmp2.v(), AF.Exp, scale=-1.0)
    k.act(tmp2.v(), tmp2.v(), AF.Ln, bias=1.0)
    k.tt(tmp.v(), tmp.v(), x.v(), ALU.add)
    k.tt(x.v(), tmp.v(), tmp2.v(), ALU.add)


@_prog_method
def conv_silu_chunk(self, wt, col0, cwname, cbname, cidx, raw, tmp, dst_v):
    k = self.k
    for j in range(NT):
        ps = self.proj(wt, col0, j)
        k.copy(raw.v(raw.h[:, j * 512:(j + 1) * 512], j), ps.v(), eng="act")
    cw = [self.pcol(cwname % kk, cidx) for kk in range(4)]
    cb = self.pcol(cbname, cidx)
    k.ts(tmp.v(), raw.v(), cw[3], ALU.mult, cb, ALU.add)
    for kk, sh in ((2, 1), (1, 2), (0, 3)):
        k.stt(tmp.v(tmp.h[:, sh:SEQ]), raw.v(raw.h[:, 0:SEQ - sh]), cw[kk], tmp.v(tmp.h[:, sh:SEQ]), ALU.mult, ALU.add)
    k.act(dst_v, tmp.v(), AF.Silu)


@_prog_method
def mixer_ssd(self):
    st = self.ssd_pre()
    for _ in self.ssd_main_gen(st):
        pass


@_prog_method
def ssd_pre(self):
    k = self.k
    dd = self.d
    W = dd["od_w_in"]
    N = NTT
    xT = k.sb([128, 4, SEQ], BF16, 4, "ssd_xT")
    BT = k.sb([128, 2, SEQ], BF16, 2, "ssd_BT")
    CT = k.sb([128, 2, SEQ], BF16, 2, "ssd_CT")
    dt = k.sb([128, N, 8], F32, 1, "ssd_dt")
    dA = k.sb([128, N, 8], F32, 1, "ssd_dA")
    cs = k.sb([128, N, 8], F32, 1, "ssd_cs")
    wcol = k.sb([128, N, 8], F32, 1, "ssd_wcol")
    dec = k.sb([128, N, 8], F32, 1, "ssd_dec")
    tri = k.sb([128, 128], F32, 1, "tri128")
    cmask = k.sb([128, 128], F32, 1, "cmask128")
    onesf = k.sb([128, 128], F32, 1, "onesf")
    k.dma("sp", tri.h[:], dd["tri128"], writes=[tri.v()])
    k.dma("sp", cmask.h[:], dd["cmask128"], writes=[cmask.v()])
    k.memset(onesf.v(), 1.0)
    with k.phase():
        wt = k.sb([128, 8, 512], BF16, 1, "w_ssd")
        raws = [k.sb([128, SEQ], F32, 4, "ssd_raw") for _ in range(2)]
        tmps = [k.sb([128, SEQ], F32, 1, "ssd_tmp") for _ in range(2)]
        wt2 = k.sb([128, 8, 512], BF16, 1, "w_ssd2")
        self.load_w(wt, W[:, 512:1024])
        self.load_w(wt2, W[:, 1024:1536])
        for c in range(4):
            self.conv_silu_chunk(wt, c * 128, "ssd_cw%d", "ssd_cb", c, raws[c % 2], tmps[c % 2], xT.v(xT.h[:, c, :], c))
        for c in range(2):
            self.conv_silu_chunk(wt2, c * 128, "ssd_cw%d", "ssd_cb", 4 + c, raws[c % 2], tmps[c % 2], BT.v(BT.h[:, c, :], c))
        for c in range(2):
            self.conv_silu_chunk(wt2, 256 + c * 128, "ssd_cw%d", "ssd_cb", 6 + c, raws[c % 2], tmps[c % 2], CT.v(CT.h[:, c, :], c))
        wdt = k.sb([128, 8, 8], BF16, 1, "w_dt")
        k.dma("pool", wdt.h[:], W[:, 1536:1544].rearrange("(kc p) n -> p kc n", p=128), writes=[wdt.v()])
        hp = k.sb([128, 3, 8], F32, 1, "ssd_hp")
        k.dma("sp", hp.h[:], dd["ssd_hp"].rearrange("(o a) h -> o a h", o=1).partition_broadcast(128), writes=[hp.v()])
        for tt in range(N):
            ps = k.ps()
            for kc in range(8):
                k.mm(ps.v(ps.h[:, 0:8]), self.HB.v(self.HB.h[:, kc, tt * 128:(tt + 1) * 128], self.rs(kc, tt // 4)),
                     wdt.v(wdt.h[:, kc, :]), start=(kc == 0), stop=(kc == 7))
            k.tt(dt.v(dt.h[:, tt, :]), ps.v(ps.h[:, 0:8]), hp.v(hp.h[:, 0, :]), ALU.add)
        t1 = k.sb([128, N, 8], F32, 1, "sp_t1")
        t2 = k.sb([128, N, 8], F32, 1, "sp_t2")
        self.softplus_inplace(dt, t1, t2)
        abc = k.sb([128, 8], F32, 1, "ssd_a")
        k.act(abc.v(), hp.v(hp.h[:, 1, :]), AF.Exp)
        k.ts(abc.v(), abc.v(), -1.0, ALU.mult)
        k.tt(dA.v(), dt.v(), abc.v(abc.h[:, :].unsqueeze(1).to_broadcast([128, N, 8])), ALU.mult)
        for tt in range(N):
            ps = k.ps()
            k.mm(ps.v(ps.h[:, 0:8]), tri.v(), dA.v(dA.h[:, tt, :]))
            k.copy(cs.v(cs.h[:, tt, :]), ps.v(ps.h[:, 0:8]), eng="act")
            ps = k.ps()
            k.mm(ps.v(ps.h[:, 0:8]), onesf.v(), dA.v(dA.h[:, tt, :]))
            k.copy(dec.v(dec.h[:, tt, :]), ps.v(ps.h[:, 0:8]), eng="act")
        k.tt(wcol.v(), dec.v(), cs.v(), ALU.subtract)
        k.act(wcol.v(), wcol.v(), AF.Exp)
        k.tt(wcol.v(), wcol.v(), dt.v(), ALU.mult)
        k.act(dec.v(), dec.v(), AF.Exp)

    return dict(xT=xT, BT=BT, CT=CT, dt=dt, dA=dA, cs=cs, wcol=wcol, dec=dec, tri=tri, cmask=cmask, onesf=onesf)


@_prog_method
def ssd_main_gen(self, st):
    k = self.k
    dd = self.d
    W = dd["od_w_in"]
    N = NTT
    xT, BT, CT, dt, dA, cs, wcol, dec, tri, cmask, onesf = (st[n_] for n_ in (
        "xT", "BT", "CT", "dt", "dA", "cs", "wcol", "dec", "tri", "cmask", "onesf"))
    w_z = k.sb([128, 8, 128], BF16, 1, "w_z")
    xtok = k.sb([128, N, 256], BF16, N, "ssd_xtok")
    Btok = k.sb([128, N, 128], BF16, N, "ssd_Btok")
    yc = k.sb([128, 2, SEQ], F32, 8, "ssd_yc")
    zs = k.sb([128, 2, SEQ], BF16, 8, "ssd_zs")
    kvall = k.sb([128, N, 4, 64], F32, N, "ssd_kvall")
    Sball = k.sb([128, N - 1, 4, 64], BF16, 1, "ssd_Sball")
    rtmp = k.sb([128, 4, 64], F32, 1, "ssd_rtmp")
    cb_sb = [k.sb([128, 4, 128], F32, 1, "cb_sb") for _ in range(2)]
    tmpw_all = k.sb([128, N, 4, 128], BF16, N, "tmpw_all")
    trs = [k.sb([128, 4, 128], F32, 1, "trs") for _ in range(2)]
    arg = [k.sb([128, 4, 128], F32, 1, "arg") for _ in range(2)]
    Wt = [k.sb([128, 4, 128], BF16, 1, "Wt") for _ in range(2)]
    ecs = [k.sb([128, 4, 128], F32, 1, "ecs") for _ in range(2)]
    CdT = [k.sb([128, 4, 128], BF16, 1, "CdT") for _ in range(2)]
    Bw = [k.sb([128, 4, 128], BF16, 1, "Bw") for _ in range(2)]
    sq = k.sb([128, 2, 512], BF16, 2, "ssd_sq")
    rst = k.sb([128, 512], F32, 1, "ssd_rst")
    B4 = [128, 4, 128]

    def bh(t, tt, h0):
        return t.v(t.h[:, tt, h0:h0 + 4].unsqueeze(2).to_broadcast(B4))

    def bi(ap):
        return ap.unsqueeze(1).to_broadcast(B4)

    for g in range(2):
        h0 = 4 * g
        for tt in range(N):
            ps = k.ps()
            pb = ps.v(ps.h[:].bitcast(BF16)[:, 0:128])
            k.tr(pb, BT.v(BT.h[:, g, tt * 128:(tt + 1) * 128], g), self.identb.v())
            k.copy(Btok.v(Btok.h[:, tt, :], tt), pb, eng="act")
            ps = k.ps()
            pb2 = ps.v(ps.h[:].bitcast(BF16)[:, 0:256])
            for q in range(2):
                c = 2 * g + q
                k.tr(ps.v(ps.h[:].bitcast(BF16)[:, q * 128:(q + 1) * 128]), xT.v(xT.h[:, c, tt * 128:(tt + 1) * 128], c),
                     self.identb.v())
            k.copy(xtok.v(xtok.h[:, tt, :], tt), pb2, eng="act")
            if tt % 4 == 3:
                yield
        for q in range(2):
            c = 2 * g + q
            k.dma("pool", w_z.h[:], W[:, c * 128:(c + 1) * 128].rearrange("(kc p) n -> p kc n", p=128), writes=[w_z.v()])
            for j in range(NT):
                ps = self.proj(w_z, 0, j)
                k.act(zs.v(zs.h[:, q, j * 512:(j + 1) * 512], q * 4 + j), ps.v(), AF.Silu)
                yield
        for tt in range(N - 1):
            b_ = tt % 2
            k.tt(Bw[b_].v(), Btok.v(bi(Btok.h[:, tt, :]), tt), bh(wcol, tt, h0), ALU.mult, eng="pool")
            pk = k.ps()
            for hq in range(4):
                k.mm(pk.v(pk.h[:, hq * 128:(hq + 1) * 128]), Bw[b_].v(Bw[b_].h[:, hq, :]),
                     xtok.v(xtok.h[:, tt, (hq // 2) * 128:(hq // 2 + 1) * 128], tt))
            pk5 = pk.h[:].rearrange("p (c a b e) -> p c a b e", c=2, a=2, b=2)
            for hp_ in range(2):
                k.copy(kvall.v(kvall.h[:, tt, hp_:4:2, :], tt), pk.v(pk5[:, :, hp_, hp_, :]), eng="act")
            if tt % 2 == 1:
                yield
        for tt in range(1, N - 1):
            k.tt(rtmp.v(), kvall.v(kvall.h[:, tt - 1, :, :], tt - 1),
                 dec.v(dec.h[:, tt, h0:h0 + 4].unsqueeze(2).to_broadcast([128, 4, 64])), ALU.mult)
            k.tt(kvall.v(kvall.h[:, tt, :, :], tt), kvall.v(kvall.h[:, tt, :, :], tt), rtmp.v(), ALU.add)
        k.copy(Sball.v(), kvall.v(kvall.h[:, 0:N - 1, :, :], range(N - 1)), eng="pool")
        yield
        for q4 in range(N // 4):
            pcb = k.ps()
            for r_ in range(4):
                tt = q4 * 4 + r_
                tok = slice(tt * 128, (tt + 1) * 128)
                k.mm(pcb.v(pcb.h[:, r_ * 128:(r_ + 1) * 128]), BT.v(BT.h[:, g, tok], g), CT.v(CT.h[:, g, tok], g))
            cbs = cb_sb[q4 % 2]
            k.tt(cbs.v(), pcb.v(pcb.h[:].rearrange("p (a b) -> p a b", a=4)),
                 cmask.v(cmask.h[:, :].unsqueeze(1).to_broadcast([128, 4, 128])), ALU.mult)
            for r_ in range(4):
                tt = q4 * 4 + r_
                k.tt(tmpw_all.v(tmpw_all.h[:, tt, :, :], tt), cbs.v(bi(cbs.h[:, r_, :])), bh(dt, tt, h0), ALU.mult,
                     eng=("pool" if r_ % 2 == 0 else "dve"))
            yield
        for tt in range(N):
            tok = slice(tt * 128, (tt + 1) * 128)
            b_ = tt % 2
            k.tt(trs[b_].v(), tri.v(bi(tri.h[:, :])), bh(dA, tt, h0), ALU.mult)
            pcs = k.ps()
            k.mm(pcs.v(), onesf.v(), trs[b_].v(trs[b_].h[:].rearrange("p a b -> p (a b)")))
            pcs4 = pcs.v(pcs.h[:].rearrange("p (a b) -> p a b", a=4))
            k.tt(arg[b_].v(), pcs4, bh(cs, tt, h0), ALU.subtract)
            k.ts(arg[b_].v(), arg[b_].v(), 0.0, ALU.min)
            k.act(arg[b_].v(), arg[b_].v(), AF.Exp)
            k.tt(Wt[b_].v(), arg[b_].v(), tmpw_all.v(tmpw_all.h[:, tt, :, :], tt), ALU.mult)
            if tt > 0:
                k.act(ecs[b_].v(), pcs4, AF.Exp)
                k.tt(CdT[b_].v(), CT.v(bi(CT.h[:, g, tok]), g), ecs[b_].v(), ALU.mult, eng="pool")
            po = k.ps()
            for hq in range(4):
                reg = po.v(po.h[:, hq * 128:(hq + 1) * 128])
                k.mm(reg, xtok.v(xtok.h[:, tt, (hq // 2) * 128:(hq // 2 + 1) * 128], tt), Wt[b_].v(Wt[b_].h[:, hq, :]),
                     start=True, stop=(tt == 0))
                if tt > 0:
                    pr = 2 * (hq // 2)
                    k.mm(reg, Sball.v(Sball.h[:, tt - 1, pr:pr + 2, :].rearrange("p a b -> p (a b)")),
                         CdT[b_].v(CdT[b_].h[:, hq, :]), start=False, stop=True)
            po4 = po.h[:].rearrange("p (c a i) -> p c a i", c=2, a=2)
            for hp_ in range(2):
                rows = slice(hp_ * 64, (hp_ + 1) * 64)
                k.copy(yc.v(yc.h[rows, :, tok], [tt // 4, 4 + tt // 4]), po.v(po4[rows, :, hp_, :]),
                       eng=("act" if hp_ == 0 else "dve"))
            yield
        for q in range(2):
            c = 2 * g + q
            sl8 = [q * 4 + j for j in range(4)]
            k.stt(yc.v(yc.h[:, q, :], sl8), xT.v(xT.h[:, c, :], c), self.pcol("ssd_dcol", c), yc.v(yc.h[:, q, :], sl8),
                  ALU.mult, ALU.add)
            k.tt(self.MIX.v(self.MIX.h[:, c, :], [self.rs(c, j) for j in range(4)]), yc.v(yc.h[:, q, :], sl8),
                 zs.v(zs.h[:, q, :], sl8), ALU.mult, eng="pool")
            yield
        for j in range(NT):
            for q in range(2):
                c = 2 * g + q
                k.act(sq.v(sq.h[:, q, :], q), self.MIXv(c, j), AF.Square)
            pm = k.ps()
            for q in range(2):
                k.mm(pm.v(), self.ones["o256"].v(), sq.v(sq.h[:, q, :], q), start=(q == 0), stop=(q == 1))
            k.act(rst.v(), pm.v(), AF.Ln, bias=self.epsv.v())
            k.act(rst.v(), rst.v(), AF.Exp, scale=-0.5)
            for q in range(2):
                c = 2 * g + q
                k.stt(self.MIXv(c, j), self.MIXv(c, j), self.pcol("ssd_nw", c), rst.v(), ALU.mult, ALU.mult)
            yield


@_prog_method
def mixer_hgrn(self):
    for _ in self.mixer_hgrn_gen():
        pass


@_prog_method
def mixer_hgrn_gen(self):
    k = self.k
    dd = self.d
    W = dd["od_w_in"]
    NCH = SEQ // 64
    lb = k.sb([128, 4], F32, 1, "hg_lb")
    oml = k.sb([128, 4], F32, 1, "hg_oml")
    k.tt(lb.v(), self.pcol("hgrn_lb1", 0, 4), self.pcol("hgrn_lb0", 0, 4), ALU.subtract)
    k.act(lb.v(), lb.v(), AF.Sigmoid)
    k.ts(oml.v(), lb.v(), -1.0, ALU.mult, 1.0, ALU.add)
    cm = k.sb([128, 128], F32, 1, "hg_cmask")
    k.dma("sp", cm.h[:], dd["cmask128"], writes=[cm.v()])
    onesf = k.sb([128, 64], F32, 1, "hg_ones")
    k.memset(onesf.v(), 1.0)
    wq = k.sb([128, 8, 128], BF16, 1, "hg_wq")
    wf = k.sb([128, 8, 128], BF16, 1, "hg_wf")
    wi = k.sb([128, 8, 128], BF16, 1, "hg_wi")
    wg = k.sb([128, 8, 128], BF16, 1, "hg_wg")
    HS = SEQ // 2
    A = k.sb([128, HS], F32, 2, "hg_A")
    B = k.sb([128, HS], F32, 2, "hg_B")
    C = k.sb([128, HS], F32, 1, "hg_C")
    E = k.sb([128, HS], F32, 1, "hg_E")
    Dq = k.sb([128, SEQ], BF16, 4, "hg_Dq")
    viT = k.sb([128, SEQ], BF16, 4, "hg_viT")
    qe = k.sb([128, SEQ], BF16, 2, "hg_qe")
    ke = k.sb([128, SEQ], BF16, 2, "hg_ke")
    qb = k.sb([128, SEQ], BF16, 2, "hg_qb")
    kend = k.sb([128, SEQ], BF16, 2, "hg_kend")
    ebe = k.sb([128, NCH], F32, 2, "hg_ebe")
    vtok = k.sb([64, NCH, 128], BF16, NCH, "hg_vtok")
    o_ts = [k.sb([128, 512], F32, 1, "hg_o")] * 2
    osq = k.sb([128, 512], BF16, 1, "hg_osq")
    rst = k.sb([128, 512], F32, 1, "hg_rst")
    sgj = k.sb([128, 512], BF16, 1, "hg_sgj")
    scm = [k.sb([64, 64], BF16, 1, "hg_scm") for _ in range(2)]
    kt8 = [k.sb([64, 8, 128], BF16, 1, "hg_kt8") for _ in range(2)]
    scm_all = k.sb([64, NCH, 64], BF16, NCH, "hg_scm_all")
    kvall = k.sb([128, NCH, 128], F32, NCH, "hg_kvall")
    Sball = k.sb([128, NCH - 1, 128], BF16, NCH // 8, "hg_Sball")
    o_ts = [k.sb([128, 512], F32, 1, "hg_o") for _ in range(2)]

    def ldw(dst, c0):
        k.dma("pool", dst.h[:], W[:, c0:c0 + 128].rearrange("(kc p) n -> p kc n", p=128), writes=[dst.v()])

    def v3(ap):
        return ap.rearrange("p (n c) -> p n c", c=64)

    NH = NCH // 2
    for hd in range(4):
        ldw(wq, 1544 + hd * 128)
        ldw(wf, 2056 + hd * 128)
        ldw(wi, 2568 + hd * 128)
        ldw(wg, 3080 + hd * 128)
        for half in range(2):
            hs = slice(half * HS, (half + 1) * HS)
            for j2 in range(2):
                j = half * 2 + j2
                loc = slice(j2 * 512, (j2 + 1) * 512)
                ps = self.proj(wf, 0, j)
                k.act(A.v(A.h[:, loc], j2), ps.v(), AF.Sigmoid)
                ps = self.proj(wq, 0, j)
                k.act(Dq.v(Dq.h[:, j * 512:(j + 1) * 512], j), ps.v(), AF.Silu)
                ps = self.proj(wi, 0, j)
                k.copy(viT.v(viT.h[:, j * 512:(j + 1) * 512], j), ps.v(), eng="dve")
                yield
            for j2 in range(2):
                loc = slice(j2 * 512, (j2 + 1) * 512)
                Aj = A.v(A.h[:, loc], j2)
                Bj = B.v(B.h[:, loc], j2)
                k.ts(Aj, Aj, oml.v(oml.h[:, hd:hd + 1]), ALU.mult, lb.v(lb.h[:, hd:hd + 1]), ALU.add)
                k.act(Bj, Aj, AF.Ln)
                k.ts(Aj, Aj, -1.0, ALU.mult, 1.0, ALU.add)
                yield
            for g8 in range(NH // 8):
                ps = k.ps()
                psb = ps.h[:].bitcast(BF16)
                n0 = half * NH + g8 * 8
                for r_ in range(8):
                    n = n0 + r_
                    ch = slice(n * 64, (n + 1) * 64)
                    k.tr(ps.v(psb[0:64, r_ * 128:(r_ + 1) * 128]), viT.v(viT.h[:, ch], n // 8), self.identb.v())
                k.copy(vtok.v(vtok.h[:, n0:n0 + 8, :], range(n0, n0 + 8)),
                       ps.v(psb[0:64, :].rearrange("p (a b) -> p a b", a=8)), eng="act")
                for r_ in range(8):
                    nl = g8 * 8 + r_
                    lc = slice(nl * 64, (nl + 1) * 64)
                    k.op("dve", lambda e, lc=lc: e.tensor_tensor_scan(out=C.h[:, lc], data0=onesf.h[:], data1=B.h[:, lc],
                                                                       initial=0.0, op0=ALU.mult, op1=ALU.add),
                         reads=[B.v(), onesf.v()], writes=[C.v()])
                yield
            C3 = v3(C.h[:, :])
            B3 = v3(B.h[:, :])
            r_b = C.v(C3[:, :, 31:32].to_broadcast([128, NH, 64]))
            be_b = C.v(C3[:, :, 63:64].to_broadcast([128, NH, 64]))
            hv = lambda t: t.v(t.h[:, hs], half)
            k.tt(B.v(B3), C.v(C3), r_b, ALU.subtract)
            k.act(E.v(), B.v(), AF.Exp)
            k.tt(hv(qe), Dq.v(Dq.h[:, hs], (2 * half, 2 * half + 1)), E.v(), ALU.mult)
            yield
            k.act(E.v(), B.v(), AF.Exp, scale=-1.0)
            k.tt(hv(ke), A.v(), E.v(), ALU.mult)
            yield
            k.act(E.v(), C.v(), AF.Exp)
            k.tt(hv(qb), Dq.v(Dq.h[:, hs], (2 * half, 2 * half + 1)), E.v(), ALU.mult, eng="pool")
            yield
            k.tt(B.v(B3), be_b, C.v(C3), ALU.subtract)
            k.act(E.v(), B.v(), AF.Exp)
            k.tt(hv(kend), A.v(), E.v(), ALU.mult, eng="pool")
            k.act(ebe.v(ebe.h[:, half * NH:(half + 1) * NH], half), C.v(C3[:, :, 63:64].rearrange("p n o -> p (n o)")), AF.Exp)
            yield
        for g8 in range(NCH // 8):
            n0 = g8 * 8
            hf = n0 // (NCH // 2)
            ps = k.ps()
            for r_ in range(8):
                ch = slice((n0 + r_) * 64, (n0 + r_ + 1) * 64)
                k.mm(ps.v(ps.h[0:64, r_ * 64:(r_ + 1) * 64]), ke.v(ke.h[:, ch], hf), qe.v(qe.h[:, ch], hf))
            k.tt(scm_all.v(scm_all.h[:, n0:n0 + 8, :], range(n0, n0 + 8)),
                 ps.v(ps.h[0:64, :].rearrange("p (a b) -> p a b", a=8)),
                 cm.v(cm.h[0:64, 0:64].unsqueeze(1).to_broadcast([64, 8, 64])), ALU.mult)
            pt = k.ps()
            ptb = pt.h[:].bitcast(BF16)
            for r_ in range(8):
                ch = slice((n0 + r_) * 64, (n0 + r_ + 1) * 64)
                k.tr(pt.v(ptb[0:64, r_ * 128:(r_ + 1) * 128]), kend.v(kend.h[:, ch], hf), self.identb.v())
            kk_ = kt8[g8 % 2]
            k.copy(kk_.v(), pt.v(ptb[0:64, :].rearrange("p (a b) -> p a b", a=8)))
            for h4 in range(2):
                pk = k.ps()
                ns = [n0 + h4 * 4 + r_ for r_ in range(4) if n0 + h4 * 4 + r_ < NCH - 1]
                for n in ns:
                    r_ = n % 4
                    k.mm(pk.v(pk.h[:, r_ * 128:(r_ + 1) * 128]), kk_.v(kk_.h[:, n - n0, :]), vtok.v(vtok.h[:, n, :], n))
                if ns:
                    k.copy(kvall.v(kvall.h[:, ns[0]:ns[0] + len(ns), :], ns),
                           pk.v(pk.h[:, 0:len(ns) * 128].rearrange("p (a b) -> p a b", a=len(ns))), eng="act")
            yield
        def recur(g8):
            for n in range(max(1, g8 * 8), min(NCH - 1, g8 * 8 + 8)):
                hf_ = n // (NCH // 2)
                k.stt(kvall.v(kvall.h[:, n, :], n), kvall.v(kvall.h[:, n - 1, :], n - 1), ebe.v(ebe.h[:, n:n + 1], hf_),
                      kvall.v(kvall.h[:, n, :], n), ALU.mult, ALU.add)
            lo, hi = g8 * 8, min(NCH - 1, g8 * 8 + 8)
            k.copy(Sball.v(Sball.h[:, lo:hi, :], g8), kvall.v(kvall.h[:, lo:hi, :], range(lo, hi)),
                   eng=("pool" if g8 % 2 == 0 else "act"))

        recur(0)
        for g8 in range(NCH // 8):
            if g8 + 1 < NCH // 8:
                recur(g8 + 1)
            n0 = g8 * 8
            hf = n0 // (NCH // 2)
            j = g8
            o_t = o_ts[g8 % 2]
            po = k.ps()
            for r_ in range(8):
                n = n0 + r_
                ch = slice(n * 64, (n + 1) * 64)
                reg = po.v(po.h[:, r_ * 64:(r_ + 1) * 64])
                k.mm(reg, vtok.v(vtok.h[:, n, :], n), scm_all.v(scm_all.h[:, n, :], n), start=True, stop=(n == 0))
                if n > 0:
                    k.mm(reg, Sball.v(Sball.h[:, n - 1, :], (n - 1) // 8), qb.v(qb.h[:, ch], hf), start=False, stop=True)
            k.copy(o_t.v(), po.v(), eng="act")
            k.act(osq.v(), o_t.v(), AF.Square)
            pm = k.ps()
            k.mm(pm.v(), self.ones["o128"].v(), osq.v())
            k.act(rst.v(), pm.v(), AF.Ln, bias=self.epsv.v())
            k.act(rst.v(), rst.v(), AF.Exp, scale=-0.5)
            ps = self.proj(wg, 0, j)
            k.act(sgj.v(), ps.v(), AF.Silu)
            k.stt(o_t.v(), o_t.v(), self.pcol("hgrn_nw", hd), rst.v(), ALU.mult, ALU.mult)
            k.tt(self.MIXv(4 + hd, j), o_t.v(), sgj.v(), ALU.mult, eng="pool")
            yield


_PROG_CACHE = {}


def build_full(pp, consts):
    cshapes = {k_: v.shape for k_, v in consts.items()}
    prog = Prog()
    prog.setup(pp.cols, pp.n, cshapes)
    prog.derived_params()
    k = prog.k
    with k.phase():
        prog.cosT = k.sb([128, SEQ], F32, 4, "cosT")
        prog.sinT = k.sb([128, SEQ], F32, 4, "sinT")
        with k.phase():
            run_interleaved([prog.load_x_gen(ALPHA, write_R=False), prog.rotary_gen()])
        prog.rot_done = True
        k.mark("load_x")
        prog.layer_mixers(0)
    prog.layer_rest(0, DEPTH == 1)
    for i in range(1, DEPTH):
        prog.layer(i, i == DEPTH - 1)
    prog.k.finish()
    return prog


def kernel(**inputs):
    shared, pp, consts = host_arrays(inputs)
    if "full" not in _PROG_CACHE:
        _PROG_CACHE["full"] = build_full(pp, consts)
    prog = _PROG_CACHE["full"]
    maps = []
    for b in range(NCORES):
        m = dict(shared)
        m.update(core_arrays(inputs, b))
        maps.append(m)
    res = run_bass_kernel_spmd(prog.k.nc, maps, core_ids=list(range(NCORES)))
    out = np.stack([np.asarray(res.results[b]["out"], dtype=np.float32) for b in range(NCORES)], axis=0)
    return out
```

```python
import math
from contextlib import contextmanager, ExitStack
import numpy as np
import concourse.bass as bass
import concourse.mybir as mybir
from concourse.bass_utils import run_bass_kernel_spmd

F32 = mybir.dt.float32
BF16 = mybir.dt.bfloat16
I32 = mybir.dt.int32
AF = mybir.ActivationFunctionType
ALU = mybir.AluOpType
AX = mybir.AxisListType

D = 1024
SEQ = 2048
NCORES = 8
DEPTH = 2
ALPHA = (2.0 * DEPTH) ** 0.25
EPS = 1e-5
NT = SEQ // 512
NTT = SEQ // 128


class T:
    def __init__(self, h, nslots=1, name=""):
        self.h = h
        self.name = name
        self.n = nslots
        self.last_w = [None] * nslots
        self.readers = [dict() for _ in range(nslots)]
        self.excl = False

    def v(self, ap=None, slots=None):
        if ap is None and self.h is not None and slots is None:
            ap = self.h[:]
        if slots is None:
            slots = range(self.n)
        elif isinstance(slots, int):
            slots = (slots,)
        return V(self, ap, tuple(slots))


class V:
    __slots__ = ("t", "ap", "slots")

    def __init__(self, t, ap, slots):
        self.t = t
        self.ap = ap
        self.slots = slots


class Eng:
    def __init__(self, name, handle, sem):
        self.name = name
        self.e = handle
        self.sem = sem
        self.count = 0
        self.known = {}
        self.snaps = [None]


class K:
    NDMA = 24

    def __init__(self):
        nc = bass.Bass("TRN2", target_bir_lowering=False)
        self.nc = nc
        self.eng = {}
        for nm, h in (("pe", nc.tensor), ("act", nc.scalar), ("dve", nc.vector),
                      ("pool", nc.gpsimd), ("sp", nc.sync)):
            self.eng[nm] = Eng(nm, h, nc.alloc_semaphore("s_" + nm))
        self.dsem = [nc.alloc_semaphore("s_dma%d" % i) for i in range(self.NDMA)]
        self.dcnt = [0] * self.NDMA
        self.dsnap = [[None] for _ in range(self.NDMA)]
        self.dnext = 0
        self.dnext_sw = 0
        self.nwaits = 0
        self.ninst = 0
        self._ps = []
        self._psi = 0
        self.uid = 0
        self._stack = None
        self.marks = []

    def sb(self, shape, dtype=F32, nslots=1, name=None):
        self.uid += 1
        name = "%s_%d" % (name or "t", self.uid)
        if self._stack is not None:
            h = self._stack.enter_context(self.nc.sbuf_tensor(name, list(shape), dtype))
            return T(h, nslots, name)
        return T(self.nc.alloc_sbuf_tensor(name, list(shape), dtype), nslots, name)

    @contextmanager
    def phase(self):
        old = self._stack
        st = ExitStack()
        self._stack = st
        try:
            yield
        finally:
            self.barrier()
            st.close()
            self._stack = old

    def psum_pool(self, n=8):
        for i in range(n):
            h = self.nc.alloc_psum_tensor("psb%d" % i, [128, 512], F32)
            t = T(h, 1, "psb%d" % i)
            t.excl = True
            self._ps.append(t)

    def ps(self):
        t = self._ps[self._psi % len(self._ps)]
        self._psi += 1
        return t

    def dram(self, name, shape, dtype=F32, kind="Internal"):
        return self.nc.dram_tensor(name, list(shape), dtype, kind=kind).ap()

    def _sem_of(self, pname):
        if pname[0] == "d" and pname[1:].isdigit():
            return self.dsem[int(pname[1:])], 16
        return self.eng[pname].sem, 1

    def _snap_of(self, pname, seq):
        if pname[0] == "d" and pname[1:].isdigit():
            return self.dsnap[int(pname[1:])][seq]
        return self.eng[pname].snaps[seq]

    def _collect(self, reads, writes):
        deps = {}
        raw = {}
        for a in reads:
            for sl in a.slots:
                p = a.t.last_w[sl]
                if p is not None:
                    if deps.get(p[0], 0) < p[1]:
                        deps[p[0]] = p[1]
                    if raw.get(p[0], 0) < p[1]:
                        raw[p[0]] = p[1]
                if a.t.excl:
                    for nm, s in a.t.readers[sl].items():
                        if deps.get(nm, 0) < s:
                            deps[nm] = s
        for a in writes:
            for sl in a.slots:
                p = a.t.last_w[sl]
                if p is not None and deps.get(p[0], 0) < p[1]:
                    deps[p[0]] = p[1]
                for nm, s in a.t.readers[sl].items():
                    if deps.get(nm, 0) < s:
                        deps[nm] = s
        return deps, raw

    def _wait(self, eng, nm, s):
        if eng.known.get(nm, 0) >= s:
            return
        sem, mult = self._sem_of(nm)
        eng.e.wait_ge(sem, s * mult)
        self.nwaits += 1
        snap = self._snap_of(nm, s)
        if snap:
            for k2, v2 in snap.items():
                if eng.known.get(k2, 0) < v2:
                    eng.known[k2] = v2
        if eng.known.get(nm, 0) < s:
            eng.known[nm] = s

    def _wait_deps(self, eng, deps, raw, same_all=False):
        for nm, s in deps.items():
            if nm == eng.name:
                if eng.name in ("pe", "sp"):
                    continue
                if not same_all:
                    s = raw.get(nm, 0)
                    if s == 0:
                        continue
            self._wait(eng, nm, s)

    def _mark(self, pname, seq, reads, writes):
        for a in reads:
            for sl in a.slots:
                r = a.t.readers[sl]
                if r.get(pname, 0) < seq:
                    r[pname] = seq
        for a in writes:
            for sl in a.slots:
                a.t.last_w[sl] = (pname, seq)
                a.t.readers[sl] = {}

    def op(self, ename, fn, reads=(), writes=(), inc=True):
        eng = self.eng[ename]
        deps, raw = self._collect(reads, writes)
        self._wait_deps(eng, deps, raw)
        ins = fn(eng.e)
        self.ninst += 1
        if not inc:
            self._mark(ename, eng.count + 1, reads, writes)
            return ins
        eng.count += 1
        ins.then_inc(eng.sem, 1)
        eng.snaps.append(dict(eng.known))
        self._mark(ename, eng.count, reads, writes)
        return ins

    def dma(self, qname, out, in_, reads=(), writes=(), **kw):
        eng = self.eng[qname]
        deps, raw = self._collect(reads, writes)
        self._wait_deps(eng, deps, raw, same_all=True)
        half = self.NDMA // 2
        if qname == "pool":
            di = half + self.dnext_sw
            self.dnext_sw = (self.dnext_sw + 1) % half
        else:
            di = self.dnext
            self.dnext = (self.dnext + 1) % half
        pname = "d%d" % di
        if self.dcnt[di] > 0:
            self._wait(eng, pname, self.dcnt[di])
        ins = eng.e.dma_start(out=out, in_=in_, **kw)
        ins.then_inc(self.dsem[di], 16)
        self.dcnt[di] += 1
        self.dsnap[di].append(dict(eng.known))
        self._mark(pname, self.dcnt[di], reads, writes)
        self.ninst += 1
        return ins

    def mark(self, name):
        self.marks.append((name, {n: e.count for n, e in self.eng.items()}))

    def barrier(self):
        names = list(self.eng.keys())
        for nm in names:
            eng = self.eng[nm]
            for nm2 in names:
                if self.eng[nm2].count > 0 and (nm2 != nm or nm in ("act", "dve", "pool")):
                    self._wait(eng, nm2, self.eng[nm2].count)
            for i in range(self.NDMA):
                if self.dcnt[i] > 0:
                    self._wait(eng, "d%d" % i, self.dcnt[i])

    def finish(self):
        sp = self.eng["sp"]
        for nm, e in self.eng.items():
            if nm != "sp" and e.count > 0:
                self._wait(sp, nm, e.count)
        for i in range(self.NDMA):
            if self.dcnt[i] > 0:
                self._wait(sp, "d%d" % i, self.dcnt[i])

    def mm(self, out, lhsT, rhs, start=True, stop=True):
        return self.op("pe", lambda e: e.matmul(out.ap, lhsT=lhsT.ap, rhs=rhs.ap, start=start, stop=stop),
                       reads=[lhsT, rhs], writes=[out], inc=bool(stop))

    def tr(self, out, in_, ident):
        return self.op("pe", lambda e: e.transpose(out.ap, in_.ap, ident.ap),
                       reads=[in_, ident], writes=[out])

    def act(self, out, in_, func, bias=None, scale=1.0):
        reads = [in_]
        kw = {}
        if isinstance(bias, V):
            reads.append(bias)
            kw["bias"] = bias.ap
        elif bias is not None:
            kw["bias"] = bias
        if isinstance(scale, V):
            reads.append(scale)
            kw["scale"] = scale.ap
        else:
            kw["scale"] = scale
        return self.op("act", lambda e: e.activation(out=out.ap, in_=in_.ap, func=func, **kw),
                       reads=reads, writes=[out])

    def tt(self, out, in0, in1, op, eng="dve"):
        return self.op(eng, lambda e: e.tensor_tensor(out=out.ap, in0=in0.ap, in1=in1.ap, op=op),
                       reads=[in0, in1], writes=[out])

    def ts(self, out, in0, s1, op0, s2=None, op1=None, eng="dve"):
        reads = [in0]
        a1 = s1
        a2 = s2
        if isinstance(s1, V):
            reads.append(s1)
            a1 = s1.ap
        if isinstance(s2, V):
            reads.append(s2)
            a2 = s2.ap
        if op1 is None:
            return self.op(eng, lambda e: e.tensor_scalar(out=out.ap, in0=in0.ap, scalar1=a1, scalar2=None, op0=op0),
                           reads=reads, writes=[out])
        return self.op(eng, lambda e: e.tensor_scalar(out=out.ap, in0=in0.ap, scalar1=a1, scalar2=a2, op0=op0, op1=op1),
                       reads=reads, writes=[out])

    def stt(self, out, in0, scalar, in1, op0, op1):
        reads = [in0, in1]
        a = scalar
        if isinstance(scalar, V):
            reads.append(scalar)
            a = scalar.ap
        return self.op("dve", lambda e: e.scalar_tensor_tensor(out=out.ap, in0=in0.ap, scalar=a, in1=in1.ap, op0=op0, op1=op1),
                       reads=reads, writes=[out])

    def copy(self, out, in_, eng="dve"):
        if eng == "act":
            return self.act(out, in_, AF.Copy)
        return self.op(eng, lambda e: e.tensor_copy(out=out.ap, in_=in_.ap), reads=[in_], writes=[out])

    def memset(self, out, val, eng="dve"):
        return self.op(eng, lambda e: e.memset(out.ap, val), reads=[], writes=[out])


RET_GAMMA_LOG = [math.log1p(-(2.0 ** (-5.0 - h))) for h in range(4)]


class PP:
    def __init__(self):
        self.cols = {}
        self.arrs = []
        self.n = 0

    def add(self, name, v):
        v = np.asarray(v, dtype=np.float32).reshape(-1)
        assert v.size % 128 == 0, (name, v.size)
        n = v.size // 128
        self.arrs.append(np.ascontiguousarray(v.reshape(n, 128).T))
        self.cols[name] = (self.n, n)
        self.n += n

    def build(self):
        return np.ascontiguousarray(np.concatenate(self.arrs, axis=1))


def make_pp(inp):
    pp = PP()
    for i in range(DEPTH):
        pp.add("ln_mix_g%d" % i, inp["ln_mix_g"][i])
        pp.add("ln_mix_b%d" % i, inp["ln_mix_b"][i])
        pp.add("ln_ffn_g%d" % i, inp["ln_ffn_g"][i])
        pp.add("ln_ffn_b%d" % i, inp["ln_ffn_b"][i])
    for kk in range(4):
        pp.add("lru_cw%d" % kk, inp["lru_conv_w"][0][kk])
    pp.add("lru_cb", inp["lru_conv_b"][0])
    pp.add("lru_br", inp["lru_b_r"][0])
    pp.add("lru_bi", inp["lru_b_i"][0])
    pp.add("lru_lam", inp["lru_lambda"][0])
    pp.add("ret_gnw", inp["ret_gn_w"][0])
    for kk in range(4):
        pp.add("ssd_cw%d" % kk, inp["ssd_conv_w"][0][kk])
    pp.add("ssd_cb", inp["ssd_conv_b"][0])
    pp.add("ssd_nw", inp["ssd_norm_w"][0])
    pp.add("hgrn_nw", inp["hgrn_norm_w"][0])
    pp.add("ssd_dcol", np.repeat(np.asarray(inp["ssd_d"][0], dtype=np.float32), 64))
    pp.add("hgrn_lb0", inp["hgrn_lb_logits"][0])
    pp.add("hgrn_lb1", inp["hgrn_lb_logits"][1])
    d = np.arange(128)
    inv = (10000.0 ** (-(np.arange(0, 128, 2, dtype=np.float32)) / 128.0)).astype(np.float32)
    pp.add("inv_freq", inv[d % 64])
    pp.add("sin_sign", np.where(d < 64, -1.0, 1.0))
    for h in range(4):
        pp.add("kdec%d" % h, np.exp(RET_GAMMA_LOG[h] * (127.0 - d)) * (128.0 ** -0.5))
    return pp


def make_consts():
    c = {}
    c["ident"] = np.eye(128, dtype=np.float32)
    j = np.arange(128)[:, None]
    i = np.arange(128)[None, :]
    cj, ci = j // 64, i // 64
    m = np.zeros((4, 128, 128), np.float32)
    for h in range(4):
        lg = RET_GAMMA_LOG[h]
        same = np.exp(lg * np.abs(i - j))
        prev = np.exp(lg * (i - j))
        m[h] = np.where(cj == ci, same, np.where(cj < ci, prev, 0.0)) * (128.0 ** -0.5)
    c["retmask"] = m
    qd = np.zeros((4, 512), np.float32)
    for h in range(4):
        qd[h] = np.tile(np.exp(RET_GAMMA_LOG[h] * (np.arange(128) + 1.0)), 4)
    c["qdec"] = qd
    cm = ((i >= j) & (cj == ci)).astype(np.float32)
    c["cmask"] = cm
    cm128 = (i >= j).astype(np.float32)
    c["cmask128"] = cm128
    sel = np.zeros((16, 16 * 128), np.float32)
    for e in range(16):
        sel[e, e * 128:(e + 1) * 128] = 1.0
    c["sel"] = sel
    c["tri"] = ((j <= i) & (cj == ci)).astype(np.float32)
    c["tri128"] = (j <= i).astype(np.float32)
    pm = np.zeros((128, 128), np.float32)
    for m_ in range(128):
        pm[(m_ + 64) % 128, m_] = 1.0
    c["perm"] = pm
    return c


WEIGHT_INPUTS = [
    ("ev_w_in", [1024, 3072]), ("ev_w_out", [1024, 1024]),
    ("od_w_in", [1024, 3592]), ("od_w_out", [1024, 1024]),
    ("lru_w_r", [8, 64, 64]), ("lru_w_i", [8, 64, 64]),
    ("moe_w_group", [2, 1024, 4]), ("moe_w_router", [2, 4, 1024, 4]),
    ("moe_w1", [2, 16, 1024, 256]), ("moe_w3", [2, 16, 1024, 256]), ("moe_w2", [2, 16, 256, 1024]),
    ("ple_w_proj", [2, 256, 1024]), ("ple_w_gate", [2, 1024, 1024]),
    ("moe_bias", [2, 20]),
    ("ssd_hp", [3, 8]),
]


class Prog:
    def __init__(self, layers=(0, 1), dbg=None, first_from_x=True, last_to_out=True):
        self.layers = layers
        self.dbg = dbg or {}
        k = self.k = K()
        nc = k.nc
        self.d = {}
        dd = self.d
        dd["x"] = k.dram("x", [SEQ, D], F32, "ExternalInput")
        dd["p"] = k.dram("p", [DEPTH, SEQ, 256], F32, "ExternalInput")
        dd["pos"] = k.dram("pos", [1, SEQ], I32, "ExternalInput")
        for nm, shp in WEIGHT_INPUTS:
            dd[nm] = k.dram(nm, shp, F32, "ExternalInput")
        self.ppinfo = None

    def setup(self, pp_cols, npp, consts_shapes):
        k = self.k
        dd = self.d
        self.pc = pp_cols
        dd["pp"] = k.dram("pp", [128, npp], F32, "ExternalInput")
        for nm, shp in consts_shapes.items():
            dd[nm] = k.dram(nm, list(shp), F32, "ExternalInput")
        dd["out"] = k.dram("out", [SEQ, D], F32, "ExternalOutput")
        for nm, (shp, dt_) in self.dbg.items():
            dd[nm] = k.dram(nm, list(shp), dt_, "ExternalOutput")
        k.psum_pool(8)
        self.R = None
        self.cosT = None
        self.sinT = None
        self.rot_done = False
        dd["r_spill"] = k.dram("r_spill", [128, 8, SEQ], F32)
        self.Rd_t = T(None, 4, "r_spill")
        self.HB = k.sb([128, 8, SEQ], BF16, 32, "HB")
        self.MIX = k.sb([128, 8, SEQ], BF16, 32, "MIX")
        self.pp = k.sb([128, npp + 64], F32, 1, "pp")
        self.ppx = npp
        self.ident = k.sb([128, 128], F32, 1, "ident")
        self.identb = k.sb([128, 128], BF16, 1, "identb")
        self.ones = {}
        self.epsv = k.sb([128, 1], F32, 1, "epsv")
        k.memset(self.epsv.v(), EPS)
        k.dma("sp", self.pp.h[:, 0:npp], dd["pp"], writes=[self.pp.v()])
        k.dma("sp", self.ident.h[:], dd["ident"], writes=[self.ident.v()])
        k.copy(self.identb.v(), self.ident.v())
        for nm, val in (("o1024", 1.0 / 1024), ("o128", 1.0 / 128), ("o256", 1.0 / 256), ("one", 1.0)):
            t = k.sb([128, 128], BF16, 1, nm)
            k.memset(t.v(), val)
            self.ones[nm] = t

    def alloc_R(self):
        self.R = self.k.sb([128, 8, SEQ], F32, 32, "R")

    def spill_R(self, j, q="sp"):
        sl = [self.rs(c, j) for c in range(8)]
        self.k.dma(q, self.d["r_spill"][:, :, j * 512:(j + 1) * 512], self.R.h[:, :, j * 512:(j + 1) * 512],
                   reads=[self.R.v(None, sl)], writes=[V(self.Rd_t, None, (j,))])

    def reload_R(self):
        for j in range(NT):
            sl = [self.rs(c, j) for c in range(8)]
            self.k.dma("sp", self.R.h[:, :, j * 512:(j + 1) * 512], self.d["r_spill"][:, :, j * 512:(j + 1) * 512],
                       reads=[V(self.Rd_t, None, (j,))], writes=[self.R.v(None, sl)])

    def pcol(self, name, j=0, n=1):
        c0, nn = self.pc[name]
        return self.pp.v(self.pp.h[:, c0 + j:c0 + j + n])

    def xcol(self, j, n=1):
        return self.pp.v(self.pp.h[:, self.ppx + j:self.ppx + j + n])

    def rs(self, c, j):
        return c * 4 + j

    def Rv(self, c, j):
        return self.R.v(self.R.h[:, c, j * 512:(j + 1) * 512], self.rs(c, j))

    def HBv(self, c, j):
        return self.HB.v(self.HB.h[:, c, j * 512:(j + 1) * 512], self.rs(c, j))

    def MIXv(self, c, j):
        return self.MIX.v(self.MIX.h[:, c, j * 512:(j + 1) * 512], self.rs(c, j))

    def dump(self, name, src_v, dram_ap=None):
        if name in self.dbg:
            self.k.dma("sp", dram_ap if dram_ap is not None else self.d[name], src_v.ap, reads=[src_v])

    def load_x(self, scale, write_R=True, write_HB=True):
        for _ in self.load_x_gen(scale, write_R, write_HB):
            pass

    def load_x_gen(self, scale, write_R=True, write_HB=True):
        k = self.k
        xt = [k.sb([128, D], F32, 1, "xtok") for _ in range(4)]
        for tt in range(NTT):
            t = xt[tt % 4]
            k.dma("sp", t.h[:], self.d["x"][tt * 128:(tt + 1) * 128, :], writes=[t.v()])
            for g in range(2):
                ps = k.ps()
                for q in range(4):
                    c = g * 4 + q
                    k.tr(ps.v(ps.h[:, q * 128:(q + 1) * 128]), t.v(t.h[:, c * 128:(c + 1) * 128]), self.ident.v())
                j = tt // 4
                sl = [self.rs(g * 4 + q, j) for q in range(4)]
                t0 = tt * 128
                pv = ps.v(ps.h[:].rearrange("p (a b) -> p a b", a=4))
                first_act = (tt * 2 + g) % 2 == 0
                if write_HB:
                    hbv = self.HB.v(self.HB.h[:, g * 4:g * 4 + 4, t0:t0 + 128], sl)
                    if first_act:
                        k.act(hbv, pv, AF.Copy)
                    else:
                        k.copy(hbv, pv)
                    first_act = not first_act
                if write_R:
                    rv = self.R.v(self.R.h[:, g * 4:g * 4 + 4, t0:t0 + 128], sl)
                    if first_act:
                        k.act(rv, pv, AF.Copy, scale=scale)
                    else:
                        k.ts(rv, pv, scale, ALU.mult)
            yield

    def load_w(self, dst, dram2d, eng="pool"):
        k = self.k
        k.dma(eng, dst.h[:], dram2d.rearrange("(kc p) n -> p kc n", p=128), writes=[dst.v()])

    def proj(self, wt, col0, j, ncols=128, kcs=8, src=None):
        k = self.k
        ps = k.ps()
        for kc in range(kcs):
            rhs = self.HBv(kc, j) if src is None else src(kc, j)
            k.mm(ps.v(ps.h[0:ncols, :]), wt.v(wt.h[:, kc, col0:col0 + ncols]), rhs, start=(kc == 0), stop=(kc == kcs - 1))
        return ps

    def mixer_lru(self):
        for _ in self.mixer_lru_gen():
            pass

    def mixer_lru_gen(self):
        k = self.k
        dd = self.d
        W = dd["ev_w_in"]
        w_xa = k.sb([128, 8, 128], BF16, 1, "w_xa")
        w_ya = k.sb([128, 8, 128], BF16, 1, "w_ya")
        wr = k.sb([128, 4, 128], BF16, 1, "wbd_r")
        wi = k.sb([128, 4, 128], BF16, 1, "wbd_i")
        k.memset(wr.v(), 0.0)
        k.memset(wi.v(), 0.0)
        for dst, src in ((wr, dd["lru_w_r"]), (wi, dd["lru_w_i"])):
            for cc in range(4):
                for hb in range(2):
                    k.dma("pool", dst.h[hb * 64:(hb + 1) * 64, cc, hb * 64:(hb + 1) * 64], src[cc * 2 + hb],
                          writes=[dst.v()])
        lam = self.pcol("lru_lam", 0, 4)
        tA = k.sb([128, 4], F32, 1, "tA")
        tB = k.sb([128, 4], F32, 1, "tB")
        nsp = k.sb([128, 4], F32, 1, "nsp")
        k.ts(tB.v(), lam, -1.0, ALU.mult, 0.0, ALU.max)
        k.stt(tA.v(), tB.v(), 2.0, lam, ALU.mult, ALU.add)
        k.act(tA.v(), tA.v(), AF.Exp, scale=-1.0)
        k.act(tA.v(), tA.v(), AF.Ln, bias=1.0)
        k.tt(tB.v(), tB.v(), tA.v(), ALU.add)
        k.ts(nsp.v(), tB.v(), -8.0, ALU.mult)

        xa = k.sb([128, SEQ], F32, 4, "xa")
        gy = k.sb([128, SEQ], BF16, 4, "gy")
        xc = k.sb([128, SEQ], F32, 1, "xc")
        xcb = k.sb([128, SEQ], BF16, 1, "xcb")
        a_t = k.sb([128, SEQ], F32, 4, "a_t")
        u_t = k.sb([128, SEQ], F32, 4, "u_t")
        hh = k.sb([128, SEQ], F32, 1, "hh")

        def sl(t, j):
            return t.v(t.h[:, j * 512:(j + 1) * 512], j)

        def ldw(dst, c0):
            k.dma("pool", dst.h[:], W[:, c0:c0 + 128].rearrange("(kc p) n -> p kc n", p=128), writes=[dst.v()])

        for cc in range(4):
            ldw(w_xa, cc * 128)
            ldw(w_ya, 512 + cc * 128)
            for j in range(NT):
                ps = self.proj(w_xa, 0, j)
                k.act(sl(xa, j), ps.v(), AF.Copy)
                yield
            for j in range(NT):
                ps = self.proj(w_ya, 0, j)
                k.act(sl(gy, j), ps.v(), AF.Gelu_apprx_tanh)
                yield
            cw = [self.pcol("lru_cw%d" % kk, cc) for kk in range(4)]
            cb = self.pcol("lru_cb", cc)
            k.ts(xc.v(), xa.v(), cw[3], ALU.mult, cb, ALU.add)
            yield
            for kk, sh in ((2, 1), (1, 2), (0, 3)):
                k.stt(xc.v(xc.h[:, sh:SEQ]), xa.v(xa.h[:, 0:SEQ - sh]), cw[kk], xc.v(xc.h[:, sh:SEQ]), ALU.mult, ALU.add)
                yield
            k.copy(xcb.v(), xc.v(), eng="pool")
            for j in range(NT):
                ps = k.ps()
                k.mm(ps.v(), wr.v(wr.h[:, cc, :]), xcb.v(xcb.h[:, j * 512:(j + 1) * 512]))
                k.act(sl(a_t, j), ps.v(), AF.Sigmoid, bias=self.pcol("lru_br", cc))
                ps = k.ps()
                k.mm(ps.v(), wi.v(wi.h[:, cc, :]), xcb.v(xcb.h[:, j * 512:(j + 1) * 512]))
                k.act(sl(u_t, j), ps.v(), AF.Sigmoid, bias=self.pcol("lru_bi", cc))
                yield
            k.act(a_t.v(), a_t.v(), AF.Exp, scale=nsp.v(nsp.h[:, cc:cc + 1]))
            yield
            k.tt(hh.v(), a_t.v(), a_t.v(), ALU.mult)
            k.act(hh.v(), hh.v(), AF.Sqrt, bias=1.0, scale=-1.0)
            yield
            k.tt(u_t.v(), u_t.v(), xc.v(), ALU.mult, eng="pool")
            k.tt(u_t.v(), u_t.v(), hh.v(), ALU.mult)
            yield
            k.op("dve", lambda e: e.tensor_tensor_scan(out=hh.h[:], data0=a_t.h[:], data1=u_t.h[:], initial=0.0,
                                                       op0=ALU.mult, op1=ALU.add),
                 reads=[a_t.v(), u_t.v()], writes=[hh.v()])
            yield
            k.tt(self.MIX.v(self.MIX.h[:, cc, :], [self.rs(cc, j) for j in range(4)]), hh.v(), gy.v(), ALU.mult, eng="pool")
            yield


def host_arrays(inp):
    pp = make_pp(inp)
    consts = make_consts()
    shared = {}
    f = lambda a: np.ascontiguousarray(np.asarray(a, dtype=np.float32))
    shared["ev_w_in"] = f(inp["ev_w_in"][0])
    shared["ev_w_out"] = f(inp["ev_w_out"][0])
    shared["od_w_in"] = f(inp["od_w_in"][0])
    shared["od_w_out"] = f(inp["od_w_out"][0])
    shared["lru_w_r"] = f(inp["lru_w_r"][0])
    shared["lru_w_i"] = f(inp["lru_w_i"][0])
    for nm in ("moe_w_group", "moe_w_router", "moe_w1", "moe_w3", "moe_w2", "ple_w_proj", "ple_w_gate"):
        shared[nm] = f(inp[nm])
    shared["moe_bias"] = f(np.concatenate([np.asarray(inp["moe_b_group"]).reshape(2, 4),
                                           np.asarray(inp["moe_b_router"]).reshape(2, 16)], axis=1))
    shared["ssd_hp"] = f(np.stack([np.asarray(inp["ssd_dt_bias"][0]), np.asarray(inp["ssd_a_log"][0]),
                                   np.asarray(inp["ssd_d"][0])], axis=0))
    shared["pp"] = pp.build()
    for nm, a in consts.items():
        shared[nm] = a
    return shared, pp, consts


def core_arrays(inp, b):
    return {
        "x": np.ascontiguousarray(np.asarray(inp["x"][b], dtype=np.float32)),
        "p": np.ascontiguousarray(np.asarray(inp["p"][:, b], dtype=np.float32)),
        "pos": np.ascontiguousarray(np.asarray(inp["positions"][b], dtype=np.int32).reshape(1, SEQ)),
    }


TWO_PI = 2.0 * math.pi
CW1 = 6.28125
CW2 = TWO_PI - CW1
PI_SAFE = 3.1415925


def _prog_method(f):
    setattr(Prog, f.__name__, f)
    return f


@_prog_method
def rotary_tables(self):
    for _ in self.rotary_gen():
        pass


@_prog_method
def rotary_gen(self):
    k = self.k
    HS = SEQ // 4
    if self.cosT is None:
        self.cosT = k.sb([128, SEQ], F32, 4, "cosT")
        self.sinT = k.sb([128, SEQ], F32, 4, "sinT")
    posi = k.sb([128, HS], I32, 1, "posi")
    ang = k.sb([128, HS], F32, 1, "ang")
    a2 = k.sb([128, HS], F32, 1, "a2")
    kf = k.sb([128, HS], F32, 1, "kf")
    for half in range(4):
        hs = slice(half * HS, (half + 1) * HS)
        k.dma("sp", posi.h[:], self.d["pos"][:, hs].partition_broadcast(128), writes=[posi.v()])
        k.copy(ang.v(), posi.v())
        k.ts(ang.v(), ang.v(), self.pcol("inv_freq"), ALU.mult)
        yield
        for shift, dst_t in ((math.pi / 2.0, self.cosT), (0.0, self.sinT)):
            dst = dst_t.v(dst_t.h[:, hs], half)
            if shift != 0.0:
                k.ts(a2.v(), ang.v(), shift, ALU.add)
                src = a2
            else:
                src = ang
            k.ts(kf.v(), src.v(), 1.0 / TWO_PI, ALU.mult)
            k.copy(posi.v(), kf.v())
            k.copy(kf.v(), posi.v())
            yield
            k.stt(dst, kf.v(), -CW1, src.v(), ALU.mult, ALU.add)
            k.stt(dst, kf.v(), -CW2, dst, ALU.mult, ALU.add)
            yield
            k.ts(kf.v(), dst, math.pi, ALU.is_gt, -TWO_PI, ALU.mult)
            k.tt(dst, dst, kf.v(), ALU.add)
            k.ts(kf.v(), dst, -math.pi, ALU.is_lt, TWO_PI, ALU.mult)
            k.tt(dst, dst, kf.v(), ALU.add)
            yield
            k.ts(dst, dst, PI_SAFE, ALU.min, -PI_SAFE, ALU.max)
            k.act(dst, dst, AF.Sin)
            if dst_t is self.sinT:
                k.ts(dst, dst, self.pcol("sin_sign"), ALU.mult)
            yield


@_prog_method
def mixer_ret(self):
    for _ in self.mixer_ret_gen():
        pass


@_prog_method
def mixer_ret_gen(self):
    k = self.k
    dd = self.d
    W = dd["ev_w_in"]
    if not self.rot_done:
        yield from self.rotary_gen()
    maskT = k.sb([128, 4, 128], F32, 1, "retmask")
    k.dma("sp", maskT.h[:], dd["retmask"].rearrange("h j i -> j h i"), writes=[maskT.v()])
    qdec = k.sb([128, 4, 128], F32, 1, "qdec")
    k.dma("sp", qdec.h[:], dd["qdec"][:, 0:128].rearrange("(o h) n -> o h n", o=1).partition_broadcast(128),
          writes=[qdec.v()])
    vtok = k.sb([128, NTT, 128], BF16, NTT, "vtok")
    wv = k.sb([128, 8, 128], BF16, 1, "wv")
    wq = k.sb([128, 8, 128], BF16, 1, "wq")
    permf = k.sb([128, 128], F32, 1, "permf")
    permb = k.sb([128, 128], BF16, 1, "permb")
    k.dma("sp", permf.h[:], dd["perm"], writes=[permf.v()])
    k.copy(permb.v(), permf.v())
    xb16 = [k.sb([128, 512], BF16, 1, "xb16")] * 2
    wk = k.sb([128, 8, 128], BF16, 1, "wk")
    wg = k.sb([128, 8, 128], BF16, 1, "wg")
    qr = k.sb([128, SEQ], BF16, 4, "qr")
    qfs = k.sb([128, SEQ], BF16, 4, "qfs")
    kr = k.sb([128, SEQ], BF16, 4, "kr")
    kte = k.sb([128, NTT, 128], BF16, NTT, "kte")
    sg = (k.sb([128, 512], BF16, 1, "sgj"), wg)
    o_ts = [k.sb([128, 512], F32, 1, "o_t")] * 2
    ob = k.sb([128, 512], BF16, 1, "ob")
    osq = k.sb([128, 512], BF16, 1, "osq")
    t1 = [k.sb([128, 512], F32, 1, "t1") for _ in range(2)]
    t2 = [k.sb([128, 512], F32, 1, "t2") for _ in range(2)]
    t3 = [k.sb([128, 512], F32, 1, "t3")] * 2
    scm4 = [k.sb([128, 4, 128], BF16, 1, "scm4") for _ in range(2)]
    kvall = k.sb([128, NTT, 128], F32, NTT, "ret_kvall")
    Sball = k.sb([128, NTT - 1, 128], BF16, 1, "ret_Sball")

    def sl(t, j):
        return t.v(t.h[:, j * 512:(j + 1) * 512], j)

    def ldw(dst, c0):
        k.dma("pool", dst.h[:], W[:, c0:c0 + 128].rearrange("(kc p) n -> p kc n", p=128), writes=[dst.v()])

    def ldw_swapped(dst, c0):
        for a, b in ((0, 64), (64, 0)):
            k.dma("pool", dst.h[:, :, a:a + 64], W[:, c0 + b:c0 + b + 64].rearrange("(kc p) n -> p kc n", p=128),
                  writes=[dst.v()])

    it = 0
    for hd in range(4):
        ldw(wq, 1024 + hd * 128)
        ldw(wk, 1536 + hd * 128)
        ldw(wg, 2560 + hd * 128)
        ldw(wv, 2048 + hd * 128)
        for tt in range(NTT):
            ps = k.ps()
            for kc in range(8):
                k.mm(ps.v(ps.h[:, 0:128]), self.HB.v(self.HB.h[:, kc, tt * 128:(tt + 1) * 128], self.rs(kc, tt // 4)),
                     wv.v(wv.h[:, kc, :]), start=(kc == 0), stop=(kc == 7))
            k.copy(vtok.v(vtok.h[:, tt, :], tt), ps.v(ps.h[:, 0:128]), eng="act")
            if tt % 2 == 1:
                yield
        for j in range(NT):
            cs = self.cosT.v(self.cosT.h[:, j * 512:(j + 1) * 512], j)
            sn = self.sinT.v(self.sinT.h[:, j * 512:(j + 1) * 512], j)
            for (w0, dst) in ((wq, qr), (wk, kr)):
                a, b = t1[it % 2], t2[it % 2]
                it += 1
                ps = self.proj(w0, 0, j)
                xb = xb16[it % 2]
                k.copy(xb.v(), ps.v(), eng="act")
                k.tt(a.v(), ps.v(), cs, ALU.mult)
                ps2 = k.ps()
                k.mm(ps2.v(), permb.v(), xb.v())
                k.tt(b.v(), ps2.v(), sn, ALU.mult)
                k.tt(sl(dst, j), a.v(), b.v(), ALU.add, eng="pool")
                if dst is qr:
                    k.tt(a.v(), a.v(), b.v(), ALU.add, eng="pool")
                    k.tt(qfs.v(qfs.h[:, j * 512:(j + 1) * 512].rearrange("p (a b) -> p a b", a=4), j),
                         a.v(a.h[:].rearrange("p (a b) -> p a b", a=4)),
                         qdec.v(qdec.h[:, hd:hd + 1, :].to_broadcast([128, 4, 128])), ALU.mult, eng="pool")
                yield
        for tt in range(NTT):
            ps = k.ps()
            pb = ps.v(ps.h[:].bitcast(BF16)[:, 0:128])
            k.tr(pb, kr.v(kr.h[:, tt * 128:(tt + 1) * 128], tt // 4), self.identb.v())
            k.ts(kte.v(kte.h[:, tt, :], tt), pb, self.pcol("kdec%d" % hd), ALU.mult)
            if tt % 4 == 3:
                yield
        g128 = math.exp(RET_GAMMA_LOG[hd] * 128.0)
        for q4 in range(NTT // 4):
            pk = k.ps()
            tts = [tt for tt in range(q4 * 4, q4 * 4 + 4) if tt < NTT - 1]
            for tt in tts:
                r_ = tt % 4
                k.mm(pk.v(pk.h[:, r_ * 128:(r_ + 1) * 128]), kte.v(kte.h[:, tt, :], tt), vtok.v(vtok.h[:, tt, :], tt))
            n_ = len(tts)
            k.copy(kvall.v(kvall.h[:, q4 * 4:q4 * 4 + n_, :], tts),
                   pk.v(pk.h[:, 0:n_ * 128].rearrange("p (a b) -> p a b", a=n_)), eng="act")
            yield
        for tt in range(1, NTT - 1):
            k.stt(kvall.v(kvall.h[:, tt, :], tt), kvall.v(kvall.h[:, tt - 1, :], tt - 1), g128,
                  kvall.v(kvall.h[:, tt, :], tt), ALU.mult, ALU.add)
        k.copy(Sball.v(), kvall.v(kvall.h[:, 0:NTT - 1, :], range(NTT - 1)), eng="pool")
        yield
        for j4 in range(NT):
            ps = k.ps()
            for r_ in range(4):
                tt = j4 * 4 + r_
                tok = slice(tt * 128, (tt + 1) * 128)
                k.mm(ps.v(ps.h[:, r_ * 128:(r_ + 1) * 128]), kr.v(kr.h[:, tok], j4), qr.v(qr.h[:, tok], j4))
            sc = scm4[j4 % 2]
            k.tt(sc.v(), ps.v(ps.h[:].rearrange("p (a b) -> p a b", a=4)),
                 maskT.v(maskT.h[:, hd:hd + 1, :].to_broadcast([128, 4, 128])), ALU.mult)
            po = k.ps()
            for r_ in range(4):
                tt = j4 * 4 + r_
                tok = slice(tt * 128, (tt + 1) * 128)
                reg = po.v(po.h[:, r_ * 128:(r_ + 1) * 128])
                k.mm(reg, vtok.v(vtok.h[:, tt, :], tt), sc.v(sc.h[:, r_, :]), start=True, stop=(tt == 0))
                if tt > 0:
                    k.mm(reg, Sball.v(Sball.h[:, tt - 1, :]), qfs.v(qfs.h[:, tok], j4), start=False, stop=True)
            o_t = o_ts[j4 % 2]
            k.copy(o_t.v(), po.v(), eng="act")
            self._ret_gn(hd, j4, o_t, sg, ob, osq, t1, t2, t3)
            yield


@_prog_method
def _ret_gn(self, hd, j, o_t, sg, ob, osq, t1, t2, t3):
    k = self.k
    oj = o_t.v()
    k.copy(ob.v(), oj, eng="act")
    k.act(osq.v(), oj, AF.Square)
    pm = k.ps()
    k.mm(pm.v(), self.ones["o128"].v(), ob.v())
    pq = k.ps()
    k.mm(pq.v(), self.ones["o128"].v(), osq.v())
    a, b, c = t1[j % 2], t2[j % 2], t3[j % 2]
    k.act(a.v(), pm.v(), AF.Square)
    k.tt(b.v(), pq.v(), a.v(), ALU.subtract)
    k.act(b.v(), b.v(), AF.Ln, bias=self.epsv.v())
    k.act(b.v(), b.v(), AF.Exp, scale=-0.5)
    k.tt(c.v(), oj, pm.v(), ALU.subtract)
    k.tt(c.v(), c.v(), b.v(), ALU.mult, eng="pool")
    sgj, wg = sg
    ps = self.proj(wg, 0, j)
    k.act(sgj.v(), ps.v(), AF.Silu)
    k.stt(self.MIXv(4 + hd, j), c.v(), self.pcol("ret_gnw", hd), sgj.v(), ALU.mult, ALU.mult)


@_prog_method
def derived_params(self):
    k = self.k
    for i in range(DEPTH):
        for w, nm in enumerate(("ln_mix_g%d" % i, "ln_mix_b%d" % i)):
            k.ts(self.xcol(i * 16 + w * 8, 8), self.pcol(nm, 0, 8), ALPHA, ALU.mult)


@_prog_method
def ln_stats(self, j, yb, ysq, tmp):
    k = self.k
    yb, ysq, tmp = yb[j % len(yb)], ysq[j % len(ysq)], tmp[j % len(tmp)]
    for c in range(8):
        k.copy(yb.v(yb.h[:, c, :], c), self.Rv(c, j), eng=("dve" if c % 2 == 0 else "pool"))
        k.act(ysq.v(ysq.h[:, c, :], c), self.Rv(c, j), AF.Square)
    pm = k.ps()
    for c in range(8):
        k.mm(pm.v(), self.ones["o1024"].v(), yb.v(yb.h[:, c, :], c), start=(c == 0), stop=(c == 7))
    pq = k.ps()
    for c in range(8):
        k.mm(pq.v(), self.ones["o1024"].v(), ysq.v(ysq.h[:, c, :], c), start=(c == 0), stop=(c == 7))
    m2, rstd, nmr, t = tmp
    k.act(m2.v(), pm.v(), AF.Square)
    k.tt(rstd.v(), pq.v(), m2.v(), ALU.subtract)
    k.act(rstd.v(), rstd.v(), AF.Ln, bias=self.epsv.v())
    k.act(rstd.v(), rstd.v(), AF.Exp, scale=-0.5)
    k.stt(nmr.v(), pm.v(), -1.0, rstd.v(), ALU.mult, ALU.mult)


@_prog_method
def ln_apply(self, j, g, b, ga, ba, tmp):
    k = self.k
    m2, rstd, nmr, t = tmp[j % len(tmp)]
    for c in range(8):
        tc_ = t[c % 2]
        k.tt(tc_.v(), self.Rv(c, j), rstd.v(), ALU.mult)
        k.tt(tc_.v(), tc_.v(), nmr.v(), ALU.add, eng="pool")
        k.ts(self.Rv(c, j), tc_.v(), ga(c), ALU.mult, ba(c), ALU.add)
        k.act(self.HBv(c, j), tc_.v(), AF.Identity, bias=b(c), scale=g(c))


@_prog_method
def ln_tile(self, j, g, b, ga, ba, yb, ysq, tmp):
    self.ln_stats(j, yb, ysq, tmp)
    self.ln_apply(j, g, b, ga, ba, tmp)


@_prog_method
def ln_bufs(self, nbuf=2, ntmp=1):
    k = self.k
    yb = [k.sb([128, 8, 512], BF16, 8, "yb") for _ in range(nbuf)]
    ysq = [k.sb([128, 8, 512], BF16, 8, "ysq") for _ in range(nbuf)]
    tmp = [(k.sb([128, 512], F32, 1, "m2"), k.sb([128, 512], F32, 1, "rstd"), k.sb([128, 512], F32, 1, "nmr"),
            [k.sb([128, 512], F32, 1, "lnt") for _ in range(2)]) for _ in range(ntmp)]
    return yb, ysq, tmp


@_prog_method
def load_wout(self, i):
    k = self.k
    Wd = self.d["ev_w_out" if i % 2 == 0 else "od_w_out"]
    self.wo = k.sb([128, 8, 1024], BF16, 1, "w_out")
    for h2 in range(2):
        k.dma("pool", self.wo.h[:, :, h2 * 512:(h2 + 1) * 512],
              Wd[:, h2 * 512:(h2 + 1) * 512].rearrange("(kc p) n -> p kc n", p=128), writes=[self.wo.v()])


@_prog_method
def outproj_ln(self, i):
    k = self.k
    wo = self.wo
    yb, ysq, tmp = self.ln_bufs(2, 2)
    g = lambda c: self.pcol("ln_mix_g%d" % i, c)
    b = lambda c: self.pcol("ln_mix_b%d" % i, c)
    ga = lambda c: self.xcol(i * 16 + c)
    ba = lambda c: self.xcol(i * 16 + 8 + c)
    for j in range(NT + 2):
        if j < NT:
            for c in range(8):
                ps = self.proj(wo, c * 128, j, src=self.MIXv)
                k.tt(self.Rv(c, j), self.Rv(c, j), ps.v(), ALU.add)
        if 0 < j <= NT:
            self.ln_stats(j - 1, yb, ysq, tmp)
        if j > 1:
            self.ln_apply(j - 2, g, b, ga, ba, tmp)
            self.router_logits(j - 2)


@_prog_method
def ln_ffn(self, i):
    k = self.k
    yb, ysq, tmp = self.ln_bufs()
    g = lambda c: self.pcol("ln_ffn_g%d" % i, c)
    b = lambda c: self.pcol("ln_ffn_b%d" % i, c)
    for j in range(NT):
        self.ln_tile(j, g, b, g, b, yb, ysq, tmp)


@_prog_method
def router_setup(self, i):
    k = self.k
    dd = self.d
    wr = k.sb([128, 8, 20], F32, 1, "wr32")
    k.dma("sp", wr.h[:, :, 0:4], dd["moe_w_group"][i].rearrange("(kc p) e -> p kc e", p=128), writes=[wr.v()])
    for g in range(4):
        k.dma("sp", wr.h[:, :, 4 + 4 * g:8 + 4 * g], dd["moe_w_router"][i, g].rearrange("(kc p) e -> p kc e", p=128),
              writes=[wr.v()])
    bias = k.sb([128, 20], F32, 1, "rbias")
    k.dma("sp", bias.h[:], dd["moe_bias"][i:i + 1, :].partition_broadcast(128), writes=[bias.v()])
    L = k.sb([128, NTT, 20], F32, NT, "L")
    self.router_st = (wr, bias, L)


@_prog_method
def router_logits(self, j):
    k = self.k
    wr, bias, L = self.router_st
    for tt in range(4 * j, 4 * j + 4):
        ps = k.ps()
        for kc in range(8):
            k.mm(ps.v(ps.h[:, 0:20]), self.R.v(self.R.h[:, kc, tt * 128:(tt + 1) * 128], self.rs(kc, tt // 4)),
                 wr.v(wr.h[:, kc, :]), start=(kc == 0), stop=(kc == 7))
        k.stt(L.v(L.h[:, tt, :], j), ps.v(ps.h[:, 0:20]), 1.0 / ALPHA, bias.v(), ALU.mult, ALU.add)


@_prog_method
def moe_route(self, i, gT):
    k = self.k
    dd = self.d
    wr, bias, L = self.router_st
    N = NTT
    GL = L.h[:, :, 0:4]
    EL = L.h[:, :, 4:20].rearrange("p t (g e) -> p t g e", g=4)
    mk = lambda shape, nm: k.sb(shape, F32, 1, nm)
    gmax = mk([128, N], "gmax")
    oh = mk([128, N, 4], "oh")
    ge = mk([128, N, 4], "ge")
    gs = mk([128, N], "gs")
    tmp4 = mk([128, N, 4, 4], "tmp4")
    ing = mk([128, N, 4], "ing")
    m1 = mk([128, N], "m1")
    k1 = mk([128, N, 4], "k1")
    ing2 = mk([128, N, 4], "ing2")
    m2 = mk([128, N], "m2")
    k2 = mk([128, N, 4], "k2")
    ed = mk([128, N], "ed")
    w1 = mk([128, N], "w1")
    w2 = mk([128, N], "w2")
    gate = mk([128, N, 4, 4], "gate")
    Lv = L.v()

    def b3(t):
        return t.v(t.h[:, :].unsqueeze(2).to_broadcast([128, N, 4]))

    def red(out, in_ap, in_t, op):
        k.op("dve", lambda e: e.tensor_reduce(out=out.h[:], in_=in_ap, axis=AX.X, op=op), reads=[in_t.v()], writes=[out.v()])

    red(gmax, GL, L, ALU.max)
    k.tt(oh.v(), L.v(GL), b3(gmax), ALU.is_equal)
    k.tt(ge.v(), L.v(GL), b3(gmax), ALU.subtract)
    k.act(ge.v(), ge.v(), AF.Exp)
    red(gs, ge.h[:], ge, ALU.add)
    k.op("dve", lambda e: e.reciprocal(out=gs.h[:], in_=gs.h[:]), reads=[gs.v()], writes=[gs.v()])
    k.tt(tmp4.v(), L.v(EL), oh.v(oh.h[:, :, :].unsqueeze(3).to_broadcast([128, N, 4, 4])), ALU.mult)
    red(ing, tmp4.h[:].rearrange("p t g e -> p t e g"), tmp4, ALU.add)
    red(m1, ing.h[:], ing, ALU.max)
    k.tt(k1.v(), ing.v(), b3(m1), ALU.is_equal)
    k.stt(ing2.v(), k1.v(), -1.0e30, ing.v(), ALU.mult, ALU.add)
    red(m2, ing2.h[:], ing2, ALU.max)
    k.tt(k2.v(), ing2.v(), b3(m2), ALU.is_equal)
    k.tt(ed.v(), m2.v(), m1.v(), ALU.subtract)
    k.act(ed.v(), ed.v(), AF.Exp)
    k.ts(w1.v(), ed.v(), 1.0, ALU.add)
    k.op("dve", lambda e: e.reciprocal(out=w1.h[:], in_=w1.h[:]), reads=[w1.v()], writes=[w1.v()])
    k.tt(w2.v(), ed.v(), w1.v(), ALU.mult)
    k.tt(w1.v(), w1.v(), gs.v(), ALU.mult)
    k.tt(w2.v(), w2.v(), gs.v(), ALU.mult)
    k.tt(k1.v(), k1.v(), b3(w1), ALU.mult)
    k.tt(k2.v(), k2.v(), b3(w2), ALU.mult)
    k.tt(k1.v(), k1.v(), k2.v(), ALU.add)
    k.tt(gate.v(), oh.v(oh.h[:, :, :].unsqueeze(3).to_broadcast([128, N, 4, 4])),
         k1.v(k1.h[:, :, :].unsqueeze(2).to_broadcast([128, N, 4, 4])), ALU.mult)
    for g4 in range(NTT // 4):
        ps = k.ps()
        for q in range(4):
            tt = g4 * 4 + q
            k.tr(ps.v(ps.h[0:16, q * 128:(q + 1) * 128]),
                 gate.v(gate.h[:, tt, :, :].rearrange("p g e -> p (g e)")), self.ident.v())
        k.copy(gT.v(gT.h[0:16, g4 * 512:(g4 + 1) * 512]), ps.v(ps.h[0:16, :]), eng="act")
    self.dump("dbg_gate", gate.v(), None)


@_prog_method
def moe(self, i):
    k = self.k
    dd = self.d
    gT = k.sb([16, SEQ], BF16, 1, "gT")
    selb = k.sb([16, 16 * 128], BF16, 1, "selb")
    w1s = [k.sb([128, 8, 256], BF16, 1, "w1e") for _ in range(2)]
    w3s = [k.sb([128, 8, 256], BF16, 1, "w3e") for _ in range(2)]
    w2s = [k.sb([128, 2, 1024], BF16, 1, "w2e") for _ in range(2)]
    NB = 4
    gB = [k.sb([128, 512], F32, 1, "gB") for _ in range(2)]
    s1 = [k.sb([128, 512], F32, 1, "s1") for _ in range(2)]
    tm = [k.sb([128, 512], F32, 1, "tm") for _ in range(2)]
    hm = [k.sb([128, 2, 512], BF16, 2, "hm") for _ in range(NB)]
    ysb = [k.sb([128, 512], F32, 1, "ysb") for _ in range(3)]

    def load_expert(e):
        w1e, w3e, w2e = w1s[e % 2], w3s[e % 2], w2s[e % 2]
        k.dma("pool", w1e.h[:], dd["moe_w1"][i, e].rearrange("(kc p) f -> p kc f", p=128), writes=[w1e.v()])
        k.dma("pool", w3e.h[:], dd["moe_w3"][i, e].rearrange("(kc p) f -> p kc f", p=128), writes=[w3e.v()])
        k.dma("pool", w2e.h[:], dd["moe_w2"][i, e].rearrange("(fc p) n -> p fc n", p=128), writes=[w2e.v()])

    def stage_a(e, j, it, part):
        w1e, w3e = w1s[e % 2], w3s[e % 2]
        gb = gB[it % 2]
        hme = hm[it % NB]
        if part == 0:
            pg = k.ps()
            k.mm(pg.v(), selb.v(selb.h[0:16, e * 128:(e + 1) * 128]), gT.v(gT.h[0:16, j * 512:(j + 1) * 512]))
            k.copy(gb.v(), pg.v(), eng="act")
        f = part
        p1 = self.proj(w1e, f * 128, j)
        p3 = self.proj(w3e, f * 128, j)
        k.act(s1[f].v(), p1.v(), AF.Silu)
        k.tt(tm[f].v(), s1[f].v(), p3.v(), ALU.mult)
        k.tt(hme.v(hme.h[:, f, :], f), tm[f].v(), gb.v(), ALU.mult, eng="pool")

    def stage_b(e, j, it, part):
        w2e = w2s[e % 2]
        hme = hm[it % NB]
        for c in range(4 * part, 4 * part + 4):
            py = k.ps()
            k.mm(py.v(), w2e.v(w2e.h[:, 0, c * 128:(c + 1) * 128]), hme.v(hme.h[:, 0, :], 0), start=True, stop=False)
            k.mm(py.v(), w2e.v(w2e.h[:, 1, c * 128:(c + 1) * 128]), hme.v(hme.h[:, 1, :], 1), start=False, stop=True)
            if c not in (1, 4, 7):
                k.tt(self.Rv(c, j), self.Rv(c, j), py.v(), ALU.add)
            else:
                yb_ = ysb[(c // 3) % len(ysb)]
                k.copy(yb_.v(), py.v(), eng="act")
                k.tt(self.Rv(c, j), self.Rv(c, j), yb_.v(), ALU.add, eng="pool")

    steps = [(e, j) for e in range(16) for j in range(NT)]
    load_expert(0)
    load_expert(1)
    k.dma("pool", selb.h[:], dd["sel"], writes=[selb.v()])
    with k.phase():
        self.moe_route(i, gT)
    for it, (e, j) in enumerate(steps):
        stage_a(e, j, it, 0)
        if it > 0:
            stage_b(steps[it - 1][0], steps[it - 1][1], it - 1, 0)
        stage_a(e, j, it, 1)
        if it > 0:
            stage_b(steps[it - 1][0], steps[it - 1][1], it - 1, 1)
        if j == 0 and 1 <= e < 15:
            load_expert(e + 1)
    stage_b(steps[-1][0], steps[-1][1], len(steps) - 1, 0)
    stage_b(steps[-1][0], steps[-1][1], len(steps) - 1, 1)


@_prog_method
def ple_prep(self, i):
    k = self.k
    dd = self.d
    PT = k.sb([128, 2, SEQ], BF16, 4, "PT")
    pt = [k.sb([128, 256], F32, 1, "ptok") for _ in range(2)]
    for g4 in range(NTT // 4):
        pss = [k.ps(), k.ps()]
        for q in range(4):
            tt = g4 * 4 + q
            t = pt[tt % 2]
            k.dma("sp", t.h[:], dd["p"][i, tt * 128:(tt + 1) * 128, :], writes=[t.v()])
            for kc in range(2):
                k.tr(pss[kc].v(pss[kc].h[:, q * 128:(q + 1) * 128]), t.v(t.h[:, kc * 128:(kc + 1) * 128]), self.ident.v())
        for kc in range(2):
            k.copy(PT.v(PT.h[:, kc, g4 * 512:(g4 + 1) * 512], g4), pss[kc].v(), eng="act")
    wg = k.sb([128, 8, 1024], BF16, 1, "w_pg")
    wp = k.sb([128, 2, 1024], BF16, 1, "w_pp")
    for h2 in range(2):
        k.dma("pool", wg.h[:, :, h2 * 512:(h2 + 1) * 512],
              dd["ple_w_gate"][i][:, h2 * 512:(h2 + 1) * 512].rearrange("(kc p) n -> p kc n", p=128), writes=[wg.v()])
    k.dma("pool", wp.h[:], dd["ple_w_proj"][i].rearrange("(kc p) n -> p kc n", p=128), writes=[wp.v()])
    sg = [k.sb([128, 512], F32, 1, "psg") for _ in range(2)]
    tq = [k.sb([128, 512], F32, 1, "ptq") for _ in range(2)]
    return PT, wg, wp, sg, tq


@_prog_method
def ple_tile(self, j, st, aout, write_hb):
    k = self.k
    PT, wg, wp, sg, tq = st
    for c in range(8):
        pgt = self.proj(wg, c * 128, j)
        pp_ = k.ps()
        for kc in range(2):
            k.mm(pp_.v(), wp.v(wp.h[:, kc, c * 128:(c + 1) * 128]), PT.v(PT.h[:, kc, j * 512:(j + 1) * 512], j),
                 start=(kc == 0), stop=(kc == 1))
        s_, t_ = sg[c % 2], tq[c % 2]
        k.act(s_.v(), pgt.v(), AF.Sigmoid)
        k.stt(t_.v(), s_.v(), aout, pp_.v(), ALU.mult, ALU.mult)
        k.stt(self.Rv(c, j), self.Rv(c, j), aout, t_.v(), ALU.mult, ALU.add)
    if write_hb:
        for c in range(8):
            k.act(self.HBv(c, j), self.Rv(c, j), AF.Copy, scale=1.0 / aout)
        self.spill_R(j)


@_prog_method
def ln_ffn_ple(self, i, aout, write_hb):
    k = self.k
    st = self.ple_prep(i)
    yb, ysq, tmp = self.ln_bufs(1, 2)
    g = lambda c: self.pcol("ln_ffn_g%d" % i, c)
    b = lambda c: self.pcol("ln_ffn_b%d" % i, c)
    for j in range(NT + 2):
        if j < NT:
            self.ln_stats(j, yb, ysq, tmp)
        if 0 < j <= NT:
            self.ln_apply(j - 1, g, b, g, b, tmp)
        if j > 1:
            self.ple_tile(j - 2, st, aout, write_hb)


@_prog_method
def store_out(self):
    k = self.k
    ot = [k.sb([128, D], F32, 1, "otok") for _ in range(2)]
    for tt in range(NTT):
        t = ot[tt % 2]
        for g in range(2):
            ps = k.ps()
            for q in range(4):
                c = g * 4 + q
                k.tr(ps.v(ps.h[:, q * 128:(q + 1) * 128]),
                     self.R.v(self.R.h[:, c, tt * 128:(tt + 1) * 128], self.rs(c, tt // 4)), self.ident.v())
            if g == 0:
                k.copy(t.v(t.h[:, 0:512]), ps.v(), eng="act")
            else:
                k.copy(t.v(t.h[:, 512:1024]), ps.v())
        k.dma("sp", self.d["out"][tt * 128:(tt + 1) * 128, :], t.h[:], reads=[t.v()])


@_prog_method
def layer(self, i, last):
    self.layer_mixers(i)
    self.layer_rest(i, last)


@_prog_method
def layer_mixers(self, i):
    k = self.k
    with k.phase():
        self.mixers(i)
    k.mark("L%d mixers" % i)


@_prog_method
def layer_rest(self, i, last):
    k = self.k
    with k.phase():
        self.alloc_R()
        self.router_setup(i)
        with k.phase():
            self.load_wout(i)
            if i == 0:
                with k.phase():
                    self.load_x(ALPHA, write_R=True, write_HB=False)
            else:
                self.reload_R()
            with k.phase():
                self.outproj_ln(i)
        k.mark("L%d outproj_ln" % i)
        with k.phase():
            self.moe(i)
        k.mark("L%d moe" % i)
        with k.phase():
            self.ln_ffn_ple(i, 1.0 if last else ALPHA, not last)
        k.mark("L%d ln_ffn_ple" % i)
        if last:
            with k.phase():
                self.store_out()
            k.mark("store")


def run_interleaved(gens):
    gens = list(gens)
    while gens:
        for g in list(gens):
            try:
                next(g)
            except StopIteration:
                gens.remove(g)


@_prog_method
def mixers(self, i):
    if i % 2 == 0:
        run_interleaved([self.mixer_ret_gen(), self.mixer_lru_gen()])
    else:
        k = self.k
        with k.phase():
            st = self.ssd_pre()
            run_interleaved([self.ssd_main_gen(st)])
        with k.phase():
            run_interleaved([self.mixer_hgrn_gen()])


@_prog_method
def softplus_inplace(self, x, tmp, tmp2):
    k = self.k
    k.ts(tmp.v(), x.v(), -1.0, ALU.mult, 0.0, ALU.max)
    k.stt(tmp2.v(), tmp.v(), 2.0, x.v(), ALU.mult, ALU.add)
    k.act(tmp2.v(), tmp2.v(), AF.Exp, scale=-1.0)
    k.act(tmp2.v(), tmp2.v(), AF.Ln, bias=1.0)
    k.tt(tmp.v(), tmp.v(), x.v(), ALU.add)
    k.tt(x.v(), tmp.v(), tmp2.v(), ALU.add)


@_prog_method
def conv_silu_chunk(self, wt, col0, cwname, cbname, cidx, raw, tmp, dst_v):
    k = self.k
    for j in range(NT):
        ps = self.proj(wt, col0, j)
        k.copy(raw.v(raw.h[:, j * 512:(j + 1) * 512], j), ps.v(), eng="act")
    cw = [self.pcol(cwname % kk, cidx) for kk in range(4)]
    cb = self.pcol(cbname, cidx)
    k.ts(tmp.v(), raw.v(), cw[3], ALU.mult, cb, ALU.add)
    for kk, sh in ((2, 1), (1, 2), (0, 3)):
        k.stt(tmp.v(tmp.h[:, sh:SEQ]), raw.v(raw.h[:, 0:SEQ - sh]), cw[kk], tmp.v(tmp.h[:, sh:SEQ]), ALU.mult, ALU.add)
    k.act(dst_v, tmp.v(), AF.Silu)


@_prog_method
def mixer_ssd(self):
    st = self.ssd_pre()
    for _ in self.ssd_main_gen(st):
        pass


@_prog_method
def ssd_pre(self):
    k = self.k
    dd = self.d
    W = dd["od_w_in"]
    N = NTT
    xT = k.sb([128, 4, SEQ], BF16, 4, "ssd_xT")
    BT = k.sb([128, 2, SEQ], BF16, 2, "ssd_BT")
    CT = k.sb([128, 2, SEQ], BF16, 2, "ssd_CT")
    dt = k.sb([128, N, 8], F32, 1, "ssd_dt")
    dA = k.sb([128, N, 8], F32, 1, "ssd_dA")
    cs = k.sb([128, N, 8], F32, 1, "ssd_cs")
    wcol = k.sb([128, N, 8], F32, 1, "ssd_wcol")
    dec = k.sb([128, N, 8], F32, 1, "ssd_dec")
    tri = k.sb([128, 128], F32, 1, "tri128")
    cmask = k.sb([128, 128], F32, 1, "cmask128")
    onesf = k.sb([128, 128], F32, 1, "onesf")
    k.dma("sp", tri.h[:], dd["tri128"], writes=[tri.v()])
    k.dma("sp", cmask.h[:], dd["cmask128"], writes=[cmask.v()])
    k.memset(onesf.v(), 1.0)
    with k.phase():
        wt = k.sb([128, 8, 512], BF16, 1, "w_ssd")
        raws = [k.sb([128, SEQ], F32, 4, "ssd_raw") for _ in range(2)]
        tmps = [k.sb([128, SEQ], F32, 1, "ssd_tmp") for _ in range(2)]
        wt2 = k.sb([128, 8, 512], BF16, 1, "w_ssd2")
        self.load_w(wt, W[:, 512:1024])
        self.load_w(wt2, W[:, 1024:1536])
        for c in range(4):
            self.conv_silu_chunk(wt, c * 128, "ssd_cw%d", "ssd_cb", c, raws[c % 2], tmps[c % 2], xT.v(xT.h[:, c, :], c))
        for c in range(2):
            self.conv_silu_chunk(wt2, c * 128, "ssd_cw%d", "ssd_cb", 4 + c, raws[c % 2], tmps[c % 2], BT.v(BT.h[:, c, :], c))
        for c in range(2):
            self.conv_silu_chunk(wt2, 256 + c * 128, "ssd_cw%d", "ssd_cb", 6 + c, raws[c % 2], tmps[c % 2], CT.v(CT.h[:, c, :], c))
        wdt = k.sb([128, 8, 8], BF16, 1, "w_dt")
        k.dma("pool", wdt.h[:], W[:, 1536:1544].rearrange("(kc p) n -> p kc n", p=128), writes=[wdt.v()])
        hp = k.sb([128, 3, 8], F32, 1, "ssd_hp")
        k.dma("sp", hp.h[:], dd["ssd_hp"].rearrange("(o a) h -> o a h", o=1).partition_broadcast(128), writes=[hp.v()])
        for tt in range(N):
            ps = k.ps()
            for kc in range(8):
                k.mm(ps.v(ps.h[:, 0:8]), self.HB.v(self.HB.h[:, kc, tt * 128:(tt + 1) * 128], self.rs(kc, tt // 4)),
                     wdt.v(wdt.h[:, kc, :]), start=(kc == 0), stop=(kc == 7))
            k.tt(dt.v(dt.h[:, tt, :]), ps.v(ps.h[:, 0:8]), hp.v(hp.h[:, 0, :]), ALU.add)
        t1 = k.sb([128, N, 8], F32, 1, "sp_t1")
        t2 = k.sb([128, N, 8], F32, 1, "sp_t2")
        self.softplus_inplace(dt, t1, t2)
        abc = k.sb([128, 8], F32, 1, "ssd_a")
        k.act(abc.v(), hp.v(hp.h[:, 1, :]), AF.Exp)
        k.ts(abc.v(), abc.v(), -1.0, ALU.mult)
        k.tt(dA.v(), dt.v(), abc.v(abc.h[:, :].unsqueeze(1).to_broadcast([128, N, 8])), ALU.mult)
        for tt in range(N):
            ps = k.ps()
            k.mm(ps.v(ps.h[:, 0:8]), tri.v(), dA.v(dA.h[:, tt, :]))
            k.copy(cs.v(cs.h[:, tt, :]), ps.v(ps.h[:, 0:8]), eng="act")
            ps = k.ps()
            k.mm(ps.v(ps.h[:, 0:8]), onesf.v(), dA.v(dA.h[:, tt, :]))
            k.copy(dec.v(dec.h[:, tt, :]), ps.v(ps.h[:, 0:8]), eng="act")
        k.tt(wcol.v(), dec.v(), cs.v(), ALU.subtract)
        k.act(wcol.v(), wcol.v(), AF.Exp)
        k.tt(wcol.v(), wcol.v(), dt.v(), ALU.mult)
        k.act(dec.v(), dec.v(), AF.Exp)

    return dict(xT=xT, BT=BT, CT=CT, dt=dt, dA=dA, cs=cs, wcol=wcol, dec=dec, tri=tri, cmask=cmask, onesf=onesf)


@_prog_method
def ssd_main_gen(self, st):
    k = self.k
    dd = self.d
    W = dd["od_w_in"]
    N = NTT
    xT, BT, CT, dt, dA, cs, wcol, dec, tri, cmask, onesf = (st[n_] for n_ in (
        "xT", "BT", "CT", "dt", "dA", "cs", "wcol", "dec", "tri", "cmask", "onesf"))
    w_z = k.sb([128, 8, 128], BF16, 1, "w_z")
    xtok = k.sb([128, N, 256], BF16, N, "ssd_xtok")
    Btok = k.sb([128, N, 128], BF16, N, "ssd_Btok")
    yc = k.sb([128, 2, SEQ], F32, 8, "ssd_yc")
    zs = k.sb([128, 2, SEQ], BF16, 8, "ssd_zs")
    kvall = k.sb([128, N, 4, 64], F32, N, "ssd_kvall")
    Sball = k.sb([128, N - 1, 4, 64], BF16, 1, "ssd_Sball")
    rtmp = k.sb([128, 4, 64], F32, 1, "ssd_rtmp")
    cb_sb = [k.sb([128, 128], F32, 1, "cb_sb") for _ in range(2)]
    tmpw = [k.sb([128, 4, 128], F32, 1, "tmpw") for _ in range(2)]
    trs = [k.sb([128, 4, 128], F32, 1, "trs") for _ in range(2)]
    arg = [k.sb([128, 4, 128], F32, 1, "arg") for _ in range(2)]
    Wt = [k.sb([128, 4, 128], BF16, 1, "Wt") for _ in range(2)]
    ecs = [k.sb([128, 4, 128], F32, 1, "ecs") for _ in range(2)]
    CdT = [k.sb([128, 4, 128], BF16, 1, "CdT") for _ in range(2)]
    Bw = [k.sb([128, 4, 128], BF16, 1, "Bw") for _ in range(2)]
    sq = k.sb([128, 2, 512], BF16, 2, "ssd_sq")
    rst = k.sb([128, 512], F32, 1, "ssd_rst")
    B4 = [128, 4, 128]

    def bh(t, tt, h0):
        return t.v(t.h[:, tt, h0:h0 + 4].unsqueeze(2).to_broadcast(B4))

    def bi(ap):
        return ap.unsqueeze(1).to_broadcast(B4)

    for g in range(2):
        h0 = 4 * g
        for tt in range(N):
            ps = k.ps()
            pb = ps.v(ps.h[:].bitcast(BF16)[:, 0:128])
            k.tr(pb, BT.v(BT.h[:, g, tt * 128:(tt + 1) * 128], g), self.identb.v())
            k.copy(Btok.v(Btok.h[:, tt, :], tt), pb, eng="act")
            ps = k.ps()
            pb2 = ps.v(ps.h[:].bitcast(BF16)[:, 0:256])
            for q in range(2):
                c = 2 * g + q
                k.tr(ps.v(ps.h[:].bitcast(BF16)[:, q * 128:(q + 1) * 128]), xT.v(xT.h[:, c, tt * 128:(tt + 1) * 128], c),
                     self.identb.v())
            k.copy(xtok.v(xtok.h[:, tt, :], tt), pb2, eng="act")
            if tt % 4 == 3:
                yield
        for q in range(2):
            c = 2 * g + q
            k.dma("pool", w_z.h[:], W[:, c * 128:(c + 1) * 128].rearrange("(kc p) n -> p kc n", p=128), writes=[w_z.v()])
            for j in range(NT):
                ps = self.proj(w_z, 0, j)
                k.act(zs.v(zs.h[:, q, j * 512:(j + 1) * 512], q * 4 + j), ps.v(), AF.Silu)
                yield
        for tt in range(N - 1):
            b_ = tt % 2
            k.tt(Bw[b_].v(), Btok.v(bi(Btok.h[:, tt, :]), tt), bh(wcol, tt, h0), ALU.mult, eng="pool")
            pk = k.ps()
            for hq in range(4):
                k.mm(pk.v(pk.h[:, hq * 128:(hq + 1) * 128]), Bw[b_].v(Bw[b_].h[:, hq, :]),
                     xtok.v(xtok.h[:, tt, (hq // 2) * 128:(hq // 2 + 1) * 128], tt))
            pk5 = pk.h[:].rearrange("p (c a b e) -> p c a b e", c=2, a=2, b=2)
            for hp_ in range(2):
                k.copy(kvall.v(kvall.h[:, tt, hp_:4:2, :], tt), pk.v(pk5[:, :, hp_, hp_, :]), eng="act")
            if tt % 2 == 1:
                yield
        for tt in range(1, N - 1):
            k.tt(rtmp.v(), kvall.v(kvall.h[:, tt - 1, :, :], tt - 1),
                 dec.v(dec.h[:, tt, h0:h0 + 4].unsqueeze(2).to_broadcast([128, 4, 64])), ALU.mult)
            k.tt(kvall.v(kvall.h[:, tt, :, :], tt), kvall.v(kvall.h[:, tt, :, :], tt), rtmp.v(), ALU.add)
        k.copy(Sball.v(), kvall.v(kvall.h[:, 0:N - 1, :, :], range(N - 1)), eng="pool")
        yield
        for tt in range(N):
            tok = slice(tt * 128, (tt + 1) * 128)
            b_ = tt % 2
            pcb = k.ps()
            k.mm(pcb.v(pcb.h[:, 0:128]), BT.v(BT.h[:, g, tok], g), CT.v(CT.h[:, g, tok], g))
            cbs = cb_sb[b_]
            k.tt(cbs.v(), pcb.v(pcb.h[:, 0:128]), cmask.v(), ALU.mult)
            k.tt(tmpw[b_].v(), cbs.v(bi(cbs.h[:, :])), bh(dt, tt, h0), ALU.mult, eng="pool")
            k.tt(trs[b_].v(), tri.v(bi(tri.h[:, :])), bh(dA, tt, h0), ALU.mult)
            pcs = k.ps()
            k.mm(pcs.v(), onesf.v(), trs[b_].v(trs[b_].h[:].rearrange("p a b -> p (a b)")))
            pcs4 = pcs.v(pcs.h[:].rearrange("p (a b) -> p a b", a=4))
            k.tt(arg[b_].v(), pcs4, bh(cs, tt, h0), ALU.subtract)
            k.ts(arg[b_].v(), arg[b_].v(), 0.0, ALU.min)
            k.act(arg[b_].v(), arg[b_].v(), AF.Exp)
            k.tt(Wt[b_].v(), arg[b_].v(), tmpw[b_].v(), ALU.mult)
            if tt > 0:
                k.act(ecs[b_].v(), pcs4, AF.Exp)
                k.tt(CdT[b_].v(), CT.v(bi(CT.h[:, g, tok]), g), ecs[b_].v(), ALU.mult, eng="pool")
            po = k.ps()
            for hq in range(4):
                reg = po.v(po.h[:, hq * 128:(hq + 1) * 128])
                k.mm(reg, xtok.v(xtok.h[:, tt, (hq // 2) * 128:(hq // 2 + 1) * 128], tt), Wt[b_].v(Wt[b_].h[:, hq, :]),
                     start=True, stop=(tt == 0))
                if tt > 0:
                    pr = 2 * (hq // 2)
                    k.mm(reg, Sball.v(Sball.h[:, tt - 1, pr:pr + 2, :].rearrange("p a b -> p (a b)")),
                         CdT[b_].v(CdT[b_].h[:, hq, :]), start=False, stop=True)
            po4 = po.h[:].rearrange("p (c a i) -> p c a i", c=2, a=2)
            for hp_ in range(2):
                rows = slice(hp_ * 64, (hp_ + 1) * 64)
                k.copy(yc.v(yc.h[rows, :, tok], [tt // 4, 4 + tt // 4]), po.v(po4[rows, :, hp_, :]),
                       eng=("act" if hp_ == 0 else "dve"))
            yield
        for q in range(2):
            c = 2 * g + q
            sl8 = [q * 4 + j for j in range(4)]
            k.stt(yc.v(yc.h[:, q, :], sl8), xT.v(xT.h[:, c, :], c), self.pcol("ssd_dcol", c), yc.v(yc.h[:, q, :], sl8),
                  ALU.mult, ALU.add)
            k.tt(self.MIX.v(self.MIX.h[:, c, :], [self.rs(c, j) for j in range(4)]), yc.v(yc.h[:, q, :], sl8),
                 zs.v(zs.h[:, q, :], sl8), ALU.mult, eng="pool")
            yield
        for j in range(NT):
            for q in range(2):
                c = 2 * g + q
                k.act(sq.v(sq.h[:, q, :], q), self.MIXv(c, j), AF.Square)
            pm = k.ps()
            for q in range(2):
                k.mm(pm.v(), self.ones["o256"].v(), sq.v(sq.h[:, q, :], q), start=(q == 0), stop=(q == 1))
            k.act(rst.v(), pm.v(), AF.Ln, bias=self.epsv.v())
            k.act(rst.v(), rst.v(), AF.Exp, scale=-0.5)
            for q in range(2):
                c = 2 * g + q
                k.stt(self.MIXv(c, j), self.MIXv(c, j), self.pcol("ssd_nw", c), rst.v(), ALU.mult, ALU.mult)
            yield


@_prog_method
def mixer_hgrn(self):
    for _ in self.mixer_hgrn_gen():
        pass


@_prog_method
def mixer_hgrn_gen(self):
    k = self.k
    dd = self.d
    W = dd["od_w_in"]
    NCH = SEQ // 64
    lb = k.sb([128, 4], F32, 1, "hg_lb")
    oml = k.sb([128, 4], F32, 1, "hg_oml")
    k.tt(lb.v(), self.pcol("hgrn_lb1", 0, 4), self.pcol("hgrn_lb0", 0, 4), ALU.subtract)
    k.act(lb.v(), lb.v(), AF.Sigmoid)
    k.ts(oml.v(), lb.v(), -1.0, ALU.mult, 1.0, ALU.add)
    cm = k.sb([128, 128], F32, 1, "hg_cmask")
    k.dma("sp", cm.h[:], dd["cmask128"], writes=[cm.v()])
    onesf = k.sb([128, 64], F32, 1, "hg_ones")
    k.memset(onesf.v(), 1.0)
    wq = k.sb([128, 8, 128], BF16, 1, "hg_wq")
    wf = k.sb([128, 8, 128], BF16, 1, "hg_wf")
    wi = k.sb([128, 8, 128], BF16, 1, "hg_wi")
    wg = k.sb([128, 8, 128], BF16, 1, "hg_wg")
    HS = SEQ // 2
    A = k.sb([128, HS], F32, 2, "hg_A")
    B = k.sb([128, HS], F32, 2, "hg_B")
    C = k.sb([128, HS], F32, 1, "hg_C")
    E = k.sb([128, HS], F32, 1, "hg_E")
    Dq = k.sb([128, SEQ], BF16, 4, "hg_Dq")
    viT = k.sb([128, SEQ], BF16, 4, "hg_viT")
    qe = k.sb([128, SEQ], BF16, 2, "hg_qe")
    ke = k.sb([128, SEQ], BF16, 2, "hg_ke")
    qb = k.sb([128, SEQ], BF16, 2, "hg_qb")
    kend = k.sb([128, SEQ], BF16, 2, "hg_kend")
    ebe = k.sb([128, NCH], F32, 2, "hg_ebe")
    vtok = k.sb([64, NCH, 128], BF16, NCH, "hg_vtok")
    o_ts = [k.sb([128, 512], F32, 1, "hg_o")] * 2
    osq = k.sb([128, 512], BF16, 1, "hg_osq")
    rst = k.sb([128, 512], F32, 1, "hg_rst")
    sgj = k.sb([128, 512], BF16, 1, "hg_sgj")
    scm = [k.sb([64, 64], BF16, 1, "hg_scm") for _ in range(2)]
    kt8 = [k.sb([64, 8, 128], BF16, 1, "hg_kt8") for _ in range(2)]
    scm_all = k.sb([64, NCH, 64], BF16, NCH, "hg_scm_all")
    kvall = k.sb([128, NCH, 128], F32, NCH, "hg_kvall")
    Sball = k.sb([128, NCH - 1, 128], BF16, NCH // 8, "hg_Sball")
    o_ts = [k.sb([128, 512], F32, 1, "hg_o") for _ in range(2)]

    def ldw(dst, c0):
        k.dma("pool", dst.h[:], W[:, c0:c0 + 128].rearrange("(kc p) n -> p kc n", p=128), writes=[dst.v()])

    def v3(ap):
        return ap.rearrange("p (n c) -> p n c", c=64)

    NH = NCH // 2
    for hd in range(4):
        ldw(wq, 1544 + hd * 128)
        ldw(wf, 2056 + hd * 128)
        ldw(wi, 2568 + hd * 128)
        ldw(wg, 3080 + hd * 128)
        for half in range(2):
            hs = slice(half * HS, (half + 1) * HS)
            for j2 in range(2):
                j = half * 2 + j2
                loc = slice(j2 * 512, (j2 + 1) * 512)
                ps = self.proj(wf, 0, j)
                k.act(A.v(A.h[:, loc], j2), ps.v(), AF.Sigmoid)
                ps = self.proj(wq, 0, j)
                k.act(Dq.v(Dq.h[:, j * 512:(j + 1) * 512], j), ps.v(), AF.Silu)
                ps = self.proj(wi, 0, j)
                k.copy(viT.v(viT.h[:, j * 512:(j + 1) * 512], j), ps.v(), eng="dve")
                yield
            for j2 in range(2):
                loc = slice(j2 * 512, (j2 + 1) * 512)
                Aj = A.v(A.h[:, loc], j2)
                Bj = B.v(B.h[:, loc], j2)
                k.ts(Aj, Aj, oml.v(oml.h[:, hd:hd + 1]), ALU.mult, lb.v(lb.h[:, hd:hd + 1]), ALU.add)
                k.act(Bj, Aj, AF.Ln)
                k.ts(Aj, Aj, -1.0, ALU.mult, 1.0, ALU.add)
                yield
            for g8 in range(NH // 8):
                ps = k.ps()
                psb = ps.h[:].bitcast(BF16)
                n0 = half * NH + g8 * 8
                for r_ in range(8):
                    n = n0 + r_
                    ch = slice(n * 64, (n + 1) * 64)
                    k.tr(ps.v(psb[0:64, r_ * 128:(r_ + 1) * 128]), viT.v(viT.h[:, ch], n // 8), self.identb.v())
                k.copy(vtok.v(vtok.h[:, n0:n0 + 8, :], range(n0, n0 + 8)),
                       ps.v(psb[0:64, :].rearrange("p (a b) -> p a b", a=8)), eng="act")
                for r_ in range(8):
                    nl = g8 * 8 + r_
                    lc = slice(nl * 64, (nl + 1) * 64)
                    k.op("dve", lambda e, lc=lc: e.tensor_tensor_scan(out=C.h[:, lc], data0=onesf.h[:], data1=B.h[:, lc],
                                                                       initial=0.0, op0=ALU.mult, op1=ALU.add),
                         reads=[B.v(), onesf.v()], writes=[C.v()])
                yield
            C3 = v3(C.h[:, :])
            B3 = v3(B.h[:, :])
            r_b = C.v(C3[:, :, 31:32].to_broadcast([128, NH, 64]))
            be_b = C.v(C3[:, :, 63:64].to_broadcast([128, NH, 64]))
            hv = lambda t: t.v(t.h[:, hs], half)
            k.tt(B.v(B3), C.v(C3), r_b, ALU.subtract)
            k.act(E.v(), B.v(), AF.Exp)
            k.tt(hv(qe), Dq.v(Dq.h[:, hs], (2 * half, 2 * half + 1)), E.v(), ALU.mult)
            yield
            k.act(E.v(), B.v(), AF.Exp, scale=-1.0)
            k.tt(hv(ke), A.v(), E.v(), ALU.mult)
            yield
            k.act(E.v(), C.v(), AF.Exp)
            k.tt(hv(qb), Dq.v(Dq.h[:, hs], (2 * half, 2 * half + 1)), E.v(), ALU.mult, eng="pool")
            yield
            k.tt(B.v(B3), be_b, C.v(C3), ALU.subtract)
            k.act(E.v(), B.v(), AF.Exp)
            k.tt(hv(kend), A.v(), E.v(), ALU.mult, eng="pool")
            k.act(ebe.v(ebe.h[:, half * NH:(half + 1) * NH], half), C.v(C3[:, :, 63:64].rearrange("p n o -> p (n o)")), AF.Exp)
            yield
        for g8 in range(NCH // 8):
            n0 = g8 * 8
            hf = n0 // (NCH // 2)
            ps = k.ps()
            for r_ in range(8):
                ch = slice((n0 + r_) * 64, (n0 + r_ + 1) * 64)
                k.mm(ps.v(ps.h[0:64, r_ * 64:(r_ + 1) * 64]), ke.v(ke.h[:, ch], hf), qe.v(qe.h[:, ch], hf))
            k.tt(scm_all.v(scm_all.h[:, n0:n0 + 8, :], range(n0, n0 + 8)),
                 ps.v(ps.h[0:64, :].rearrange("p (a b) -> p a b", a=8)),
                 cm.v(cm.h[0:64, 0:64].unsqueeze(1).to_broadcast([64, 8, 64])), ALU.mult)
            pt = k.ps()
            ptb = pt.h[:].bitcast(BF16)
            for r_ in range(8):
                ch = slice((n0 + r_) * 64, (n0 + r_ + 1) * 64)
                k.tr(pt.v(ptb[0:64, r_ * 128:(r_ + 1) * 128]), kend.v(kend.h[:, ch], hf), self.identb.v())
            kk_ = kt8[g8 % 2]
            k.copy(kk_.v(), pt.v(ptb[0:64, :].rearrange("p (a b) -> p a b", a=8)))
            for h4 in range(2):
                pk = k.ps()
                ns = [n0 + h4 * 4 + r_ for r_ in range(4) if n0 + h4 * 4 + r_ < NCH - 1]
                for n in ns:
                    r_ = n % 4
                    k.mm(pk.v(pk.h[:, r_ * 128:(r_ + 1) * 128]), kk_.v(kk_.h[:, n - n0, :]), vtok.v(vtok.h[:, n, :], n))
                if ns:
                    k.copy(kvall.v(kvall.h[:, ns[0]:ns[0] + len(ns), :], ns),
                           pk.v(pk.h[:, 0:len(ns) * 128].rearrange("p (a b) -> p a b", a=len(ns))), eng="act")
            yield
        def recur(g8):
            for n in range(max(1, g8 * 8), min(NCH - 1, g8 * 8 + 8)):
                hf_ = n // (NCH // 2)
                k.stt(kvall.v(kvall.h[:, n, :], n), kvall.v(kvall.h[:, n - 1, :], n - 1), ebe.v(ebe.h[:, n:n + 1], hf_),
                      kvall.v(kvall.h[:, n, :], n), ALU.mult, ALU.add)
            lo, hi = g8 * 8, min(NCH - 1, g8 * 8 + 8)
            k.copy(Sball.v(Sball.h[:, lo:hi, :], g8), kvall.v(kvall.h[:, lo:hi, :], range(lo, hi)),
                   eng=("pool" if g8 % 2 == 0 else "act"))

        recur(0)
        for g8 in range(NCH // 8):
            if g8 + 1 < NCH // 8:
                recur(g8 + 1)
            n0 = g8 * 8
            hf = n0 // (NCH // 2)
            j = g8
            o_t = o_ts[g8 % 2]
            po = k.ps()
            for r_ in range(8):
                n = n0 + r_
                ch = slice(n * 64, (n + 1) * 64)
                reg = po.v(po.h[:, r_ * 64:(r_ + 1) * 64])
                k.mm(reg, vtok.v(vtok.h[:, n, :], n), scm_all.v(scm_all.h[:, n, :], n), start=True, stop=(n == 0))
                if n > 0:
                    k.mm(reg, Sball.v(Sball.h[:, n - 1, :], (n - 1) // 8), qb.v(qb.h[:, ch], hf), start=False, stop=True)
            k.copy(o_t.v(), po.v(), eng="act")
            k.act(osq.v(), o_t.v(), AF.Square)
            pm = k.ps()
            k.mm(pm.v(), self.ones["o128"].v(), osq.v())
            k.act(rst.v(), pm.v(), AF.Ln, bias=self.epsv.v())
            k.act(rst.v(), rst.v(), AF.Exp, scale=-0.5)
            ps = self.proj(wg, 0, j)
            k.act(sgj.v(), ps.v(), AF.Silu)
            k.stt(o_t.v(), o_t.v(), self.pcol("hgrn_nw", hd), rst.v(), ALU.mult, ALU.mult)
            k.tt(self.MIXv(4 + hd, j), o_t.v(), sgj.v(), ALU.mult, eng="pool")
            yield


_PROG_CACHE = {}


def build_full(pp, consts):
    cshapes = {k_: v.shape for k_, v in consts.items()}
    prog = Prog()
    prog.setup(pp.cols, pp.n, cshapes)
    prog.derived_params()
    k = prog.k
    with k.phase():
        prog.cosT = k.sb([128, SEQ], F32, 4, "cosT")
        prog.sinT = k.sb([128, SEQ], F32, 4, "sinT")
        with k.phase():
            run_interleaved([prog.load_x_gen(ALPHA, write_R=False), prog.rotary_gen()])
        prog.rot_done = True
        k.mark("load_x")
        prog.layer_mixers(0)
    prog.layer_rest(0, DEPTH == 1)
    for i in range(1, DEPTH):
        prog.layer(i, i == DEPTH - 1)
    prog.k.finish()
    return prog


def kernel(**inputs):
    shared, pp, consts = host_arrays(inputs)
    if "full" not in _PROG_CACHE:
        _PROG_CACHE["full"] = build_full(pp, consts)
    prog = _PROG_CACHE["full"]
    maps = []
    for b in range(NCORES):
        m = dict(shared)
        m.update(core_arrays(inputs, b))
        maps.append(m)
    res = run_bass_kernel_spmd(prog.k.nc, maps, core_ids=list(range(NCORES)))
    out = np.stack([np.asarray(res.results[b]["out"], dtype=np.float32) for b in range(NCORES)], axis=0)
    return out
```
